# Optimizing a Trainium2 kernel written in Bass

```python
import math
import jax, jax.numpy as jnp
from jax import lax
import numpy as np

D_MODEL = 1024
BATCH = 4
SEQ = 4096
DEPTH = 1

EPS = 1e-6
GMLP_WIDTH = 512
GMLP_GROUPS = 8
CHUNK = 128
N_HEADS = 8
HEAD_DIM = 64
ATTN_WIDTH = N_HEADS * HEAD_DIM
DILATED_PATTERNS = ((128, 1), (512, 4), (2048, 16))
Q_BLOCK = 128
N_EXPERTS = 16
EXPERT_HIDDEN = 1024
CAPACITY_FACTOR = 2
IN_COLS = 2 * GMLP_WIDTH + 3 * ATTN_WIDTH + 2 * D_MODEL

kernel_name = "hybrid_gmlp_dilated_attn_ec_moe"


def _rmsnorm(x, g):
    xf = x.astype(jnp.float32)
    y = xf * lax.rsqrt(jnp.mean(xf * xf, axis=-1, keepdims=True) + EPS)
    return (y * g.astype(jnp.float32)).astype(x.dtype)


def _gmlp_branch(u, v, gmlp_norm_g, w_spatial, b_spatial):
    B, S, _ = v.shape
    gd = GMLP_WIDTH // GMLP_GROUPS
    v = _rmsnorm(v, gmlp_norm_g)
    vc = v.reshape(B, S // CHUNK, CHUNK, GMLP_GROUPS, gd)
    mixed = jnp.einsum('gts,bcsgd->bctgd', w_spatial.astype(v.dtype), vc)
    mixed = mixed + b_spatial.T.astype(v.dtype)[None, None, :, :, None]
    return u * mixed.reshape(B, S, GMLP_WIDTH)


def _dilated_attention(q, k, v, slopes):
    B, H, S, hd = q.shape
    scale = hd ** -0.5
    n_blocks = S // Q_BLOCK

    def block(i):
        q0 = i * Q_BLOCK
        qb = lax.dynamic_slice_in_dim(q, q0, Q_BLOCK, axis=2).astype(jnp.float32)
        t = q0 + jnp.arange(Q_BLOCK)
        outs, lses = [], []
        for win, dil in DILATED_PATTERNS:
            half = win // (2 * dil)
            offs = dil * jnp.arange(-half, half + 1)
            pos = t[:, None] + offs[None, :]
            valid = (pos >= 0) & (pos < S)
            pos_c = jnp.clip(pos, 0, S - 1)
            kb = jnp.take(k, pos_c, axis=2).astype(jnp.float32)
            vb = jnp.take(v, pos_c, axis=2).astype(jnp.float32)
            s = jnp.einsum('bhqd,bhqkd->bhqk', qb, kb) * scale
            s = s - slopes[None, :, None, None] * jnp.abs(offs).astype(jnp.float32)[None, None, None, :]
            s = jnp.where(valid[None, None], s, -jnp.inf)
            m = jnp.max(s, axis=-1, keepdims=True)
            p = jnp.exp(s - m)
            den = jnp.sum(p, axis=-1)
            o = jnp.einsum('bhqk,bhqkd->bhqd', p, vb) / den[..., None]
            outs.append(o)
            lses.append(m[..., 0] + jnp.log(den))
        w = jax.nn.softmax(jnp.stack(lses, axis=0), axis=0)
        return jnp.sum(w[..., None] * jnp.stack(outs, axis=0), axis=0)

    o = lax.map(block, jnp.arange(n_blocks))
    o = o.transpose(1, 2, 0, 3, 4).reshape(B, H, S, hd)
    return o.astype(q.dtype)


def _expert_choice_moe(h, w_router, w_e_gate, w_e_up, w_e_down):
    B, S, D = h.shape
    cap = CAPACITY_FACTOR * S // N_EXPERTS
    logits = jnp.einsum('bsd,de->bse', h, w_router).astype(jnp.float32)
    aff = jax.nn.softmax(logits, axis=-1)
    gate, idx = lax.top_k(aff.transpose(0, 2, 1), cap)
    xe = jax.vmap(lambda hb, ib: hb[ib])(h, idx)
    g = jnp.einsum('becd,edf->becf', xe, w_e_gate)
    up = jnp.einsum('becd,edf->becf', xe, w_e_up)
    ye = jnp.einsum('becf,efd->becd', jax.nn.silu(g) * up, w_e_down)
    ye = ye * gate.astype(ye.dtype)[..., None]
    out = jax.vmap(lambda ib, yb: jnp.zeros((S, D), yb.dtype).at[ib.reshape(-1)].add(yb.reshape(-1, D)))(idx, ye)
    return out


def setup_inputs(seed: int = 0) -> dict:
    key = jax.random.key(seed)
    ks = jax.random.split(key, 20)
    f32 = jnp.float32
    L, D = DEPTH, D_MODEL
    nrm = lambda k, shape, fan_in: jax.random.normal(k, shape, f32) * (fan_in ** -0.5)
    gain = lambda k, shape: 1.0 + 0.05 * jax.random.normal(k, shape, f32)
    return {
        "x": jax.random.normal(ks[0], (BATCH, SEQ, D), f32),
        "norm_mix_g": gain(ks[1], (L, D)),
        "w_in": nrm(ks[2], (L, D, IN_COLS), D),
        "b_gate": 0.02 * jax.random.normal(ks[3], (L, 2 * D), f32),
        "gmlp_norm_g": gain(ks[4], (L, GMLP_WIDTH)),
        "w_spatial": nrm(ks[5], (L, GMLP_GROUPS, CHUNK, CHUNK), CHUNK),
        "b_spatial": gain(ks[6], (L, GMLP_GROUPS, CHUNK)),
        "w_proj_a": nrm(ks[7], (L, GMLP_WIDTH, D), GMLP_WIDTH),
        "w_proj_b": nrm(ks[8], (L, ATTN_WIDTH, D), ATTN_WIDTH),
        "w_out": nrm(ks[9], (L, D, D), D),
        "norm_ffn_g": gain(ks[10], (L, D)),
        "w_router": nrm(ks[11], (L, D, N_EXPERTS), D),
        "w_e_gate": nrm(ks[12], (L, N_EXPERTS, D, EXPERT_HIDDEN), D),
        "w_e_up": nrm(ks[13], (L, N_EXPERTS, D, EXPERT_HIDDEN), D),
        "w_e_down": nrm(ks[14], (L, N_EXPERTS, EXPERT_HIDDEN, D), EXPERT_HIDDEN),
        "norm_final_g": gain(ks[15], (D,)),
    }


def reference(x, norm_mix_g, w_in, b_gate, gmlp_norm_g, w_spatial, b_spatial, w_proj_a, w_proj_b,
              w_out, norm_ffn_g, w_router, w_e_gate, w_e_up, w_e_down, norm_final_g):
    B, S, D = x.shape
    slopes = 2.0 ** (-8.0 * jnp.arange(1, N_HEADS + 1, dtype=jnp.float32) / N_HEADS)
    splits = np.cumsum([GMLP_WIDTH, GMLP_WIDTH, ATTN_WIDTH, ATTN_WIDTH, ATTN_WIDTH, D_MODEL]).tolist()
    for l in range(DEPTH):
        h = _rmsnorm(x, norm_mix_g[l])
        proj = jnp.einsum('bsd,dc->bsc', h, w_in[l])
        u, v, q, k, va, ga, gb = jnp.split(proj, splits, axis=-1)
        ga = jax.nn.sigmoid(ga + b_gate[l, :D].astype(ga.dtype))
        gb = jax.nn.sigmoid(gb + b_gate[l, D:].astype(gb.dtype))
        a = _gmlp_branch(jax.nn.gelu(u), jax.nn.gelu(v), gmlp_norm_g[l], w_spatial[l], b_spatial[l])
        to_heads = lambda t: t.reshape(B, S, N_HEADS, HEAD_DIM).transpose(0, 2, 1, 3)
        o = _dilated_attention(to_heads(q), to_heads(k), to_heads(va), slopes)
        o = o.transpose(0, 2, 1, 3).reshape(B, S, ATTN_WIDTH)
        merged = ga * jnp.einsum('bsc,cd->bsd', a, w_proj_a[l]) + gb * jnp.einsum('bsc,cd->bsd', o, w_proj_b[l])
        x = x + jnp.einsum('bsd,de->bse', merged, w_out[l])
        h2 = _rmsnorm(x, norm_ffn_g[l])
        x = x + _expert_choice_moe(h2, w_router[l], w_e_gate[l], w_e_up[l], w_e_down[l])
    return _rmsnorm(x, norm_final_g)
```

```python
import contextlib
import numpy as np
import concourse.bass as bass
import concourse.mybir as mybir
from concourse.bass_utils import run_bass_kernel_spmd

F32 = mybir.dt.float32
BF16 = mybir.dt.bfloat16
I32 = mybir.dt.int32
AF = mybir.ActivationFunctionType
ALU = mybir.AluOpType
AX = mybir.AxisListType

SEQ = 4096
D = 1024
NT = SEQ // 128
INC = 4608
NE = 16
CAP = 512
ROW = 1024
EPS = 1e-6
PATTERNS = (1, 4, 16)
NPOOL = 12
SKEW = 2
DEBUG = None


class S:
    def __init__(self, nc, es):
        self.nc = nc
        self.e = {"pe": nc.tensor, "act": nc.scalar, "dve": nc.vector, "pool": nc.gpsimd, "sp": nc.sync}
        self.sem = {}
        for k in ("pe", "act", "dve", "pool"):
            self.sem[k] = es.enter_context(nc.semaphore("s_" + k))
        self.cnt = {k: 0 for k in ("pe", "act", "dve", "pool")}
        self.dsem = {}
        self.duse = {}
        self.drr = {"sp": 0, "pool": 0, "act": 0}
        for q in ("sp", "pool", "act"):
            for i in range(NPOOL):
                self.dsem[(q, i)] = es.enter_context(nc.semaphore("d_%s%d" % (q, i)))
                self.duse[(q, i)] = 0
        self.known = {k: {} for k in self.e}
        self.lw = {}
        self.rd = {}

    def _semh(self, key):
        return self.sem[key] if key in self.sem else self.dsem[key]

    def _wait(self, eng, deps):
        need = {}
        for (k, v) in deps:
            if k == eng:
                if eng == "pe":
                    continue
            if v > need.get(k, 0):
                need[k] = v
        kn = self.known[eng]
        for k, v in need.items():
            if kn.get(k, 0) >= v:
                continue
            self.e[eng].wait_ge(self._semh(k), v)
            kn[k] = v

    def _deps(self, r, w):
        deps = []
        for k in r:
            if k in self.lw:
                deps.append(self.lw[k])
        for k in w:
            if k in self.lw:
                deps.append(self.lw[k])
            deps.extend(self.rd.get(k, {}).items())
        return deps

    def _commit(self, ev, r, w):
        for k in r:
            d = self.rd.setdefault(k, {})
            if ev[1] > d.get(ev[0], 0):
                d[ev[0]] = ev[1]
        for k in w:
            self.lw[k] = ev
            self.rd[k] = {}

    def op(self, eng, fn, r=(), w=()):
        self._wait(eng, self._deps(r, w))
        ins = fn(self.e[eng])
        self.cnt[eng] += 1
        ins.then_inc(self.sem[eng], 1)
        ev = (eng, self.cnt[eng])
        self._commit(ev, r, w)
        return ev

    def mm(self, out, lhsT, rhs, start=True, stop=True, r=(), w=(), skip=False):
        return self.op("pe", lambda e: e.matmul(out, lhsT=lhsT, rhs=rhs, start=start, stop=stop, skip_group_check=skip), r, w)

    def tr(self, out, in_, ident, r=(), w=()):
        return self.op("pe", lambda e: e.transpose(out, in_, ident), r, w)

    def dma(self, q, fn, r=(), w=()):
        i = self.drr[q]
        self.drr[q] = (i + 1) % NPOOL
        key = (q, i)
        deps = self._deps(r, w)
        if self.duse[key] > 0:
            deps.append((key, 16 * self.duse[key]))
        self._wait(q, deps)
        ins = fn(self.e[q])
        self.duse[key] += 1
        ins.then_inc(self.dsem[key], 16)
        ev = (key, 16 * self.duse[key])
        self._commit(ev, r, w)
        return ev

    def barrier(self):
        for eng in ("pe", "act", "dve", "pool", "sp"):
            deps = [(k, self.cnt[k]) for k in self.cnt if k != eng and self.cnt[k] > 0]
            deps += [(k, 16 * u) for k, u in self.duse.items() if u > 0]
            kn = self.known[eng]
            for k, v in deps:
                if kn.get(k, 0) >= v:
                    continue
                self.e[eng].wait_ge(self._semh(k), v)
                kn[k] = v
        self.lw = {}
        self.rd = {}


def tokv(ap, r, d, m0, n):
    if d == 1:
        return ap[:, m0:m0 + n]
    return ap[:, r + d * m0: r + d * (m0 + n - 1) + 1: d]


class _Stop(Exception):
    pass


def build():
    try:
        return _build()
    except _Stop as e:
        return e.args[0]


def _build():
    nc = bass.Bass("TRN2", target_bir_lowering=False)
    dt = nc.dram_tensor
    x_d = dt("x", [SEQ, D], F32, kind="ExternalInput").ap()
    gmix_d = dt("norm_mix_g", [1, D], F32, kind="ExternalInput").ap()
    win_d = dt("w_in", [D, INC], F32, kind="ExternalInput").ap()
    bgate_d = dt("b_gate", [16, 128], F32, kind="ExternalInput").ap()
    ggm_d = dt("gmlp_norm_g", [1, 512], F32, kind="ExternalInput").ap()
    wsp_d = dt("w_spatial", [8, 128, 128], F32, kind="ExternalInput").ap()
    bsp_d = dt("b_spatial", [8, 128], F32, kind="ExternalInput").ap()
    wpa_d = dt("w_proj_a", [512, D], F32, kind="ExternalInput").ap()
    wpb_d = dt("w_proj_b", [512, D], F32, kind="ExternalInput").ap()
    wout_d = dt("w_out", [D, D], F32, kind="ExternalInput").ap()
    gffn_d = dt("norm_ffn_g", [1, D], F32, kind="ExternalInput").ap()
    wr_d = dt("w_router", [D, NE], F32, kind="ExternalInput").ap()
    weg_d = dt("w_e_gate", [NE, D, D], F32, kind="ExternalInput").ap()
    weu_d = dt("w_e_up", [NE, D, D], F32, kind="ExternalInput").ap()
    wed_d = dt("w_e_down", [NE, D, D], F32, kind="ExternalInput").ap()
    gfin_d = dt("norm_final_g", [1, D], F32, kind="ExternalInput").ap()
    out_d = dt("out", [SEQ, D], F32, kind="ExternalOutput").ap()
    hT_d = dt("hT_scr", [128, 8, SEQ], BF16, kind="Internal").ap()
    x1_d = dt("x1_scr", [SEQ, D], F32, kind="Internal").ap()
    h2_d = dt("h2_scr", [SEQ, ROW], BF16, kind="Internal").ap()
    aff_d = dt("aff_scr", [SEQ, NE], F32, kind="Internal").ap()
    dbg_d = None
    if DEBUG:
        dbg_d = dt("dbg", [128, 4 * SEQ], F32, kind="ExternalOutput").ap()

    with contextlib.ExitStack() as es:
        s = S(nc, es)

        def ck(n):
            if DEBUG == "p1:%d" % n:
                s.barrier()
                raise _Stop(nc)

        def sb(name, shape, dtype, stack=es):
            return stack.enter_context(nc.sbuf_tensor(name, shape, dtype))

        PSW = [es.enter_context(nc.psum_tensor("psw%d" % i, [128, 1024], F32)) for i in range(4)]
        PS = [PSW[i // 2][:, (i % 2) * 512:(i % 2 + 1) * 512] for i in range(8)]

        def psb(i):
            return PS[i][:, :].bitcast(BF16)

        identb = sb("identb", [128, 128], BF16)
        identf = sb("identf", [128, 128], F32)
        onesb = sb("onesb", [128, 128], BF16)
        iot = sb("iot", [128, 128], F32)
        nhalf = sb("nhalf", [128, 1], F32)
        idxi = sb("idxi", [128, NE, 4], I32)
        st_aff = contextlib.ExitStack()
        affT = sb("affT", [16, SEQ], F32, st_aff)
        st_oT = contextlib.ExitStack()
        oT = sb("oT", [128, 4, SEQ], BF16, st_oT)

        s.op("pool", lambda e: e.iota(iot[:], pattern=[[1, 128]], base=0, channel_multiplier=-1,
                                      allow_small_or_imprecise_dtypes=True), w=["iot"])
        s.op("dve", lambda e: e.tensor_scalar(out=identf[:], in0=iot[:], scalar1=0.0, scalar2=None, op0=ALU.is_equal),
             r=["iot"], w=["identf"])
        s.op("dve", lambda e: e.tensor_copy(out=identb[:], in_=identf[:]), r=["identf"], w=["identb"])
        s.op("pool", lambda e: e.memset(onesb[:], 1.0), w=["onesb"])
        s.op("pool", lambda e: e.memset(nhalf[:], -0.5), w=["nhalf"])

        def rstd_from_ssq(ssq, ms, rstd, n, tag):
            s.op("dve", lambda e: e.tensor_scalar(out=ms, in0=ssq, scalar1=1.0 / n, scalar2=EPS, op0=ALU.mult, op1=ALU.add),
                 r=[tag + "ssq"], w=[tag + "ms"])
            s.op("pool", lambda e: e.tensor_tensor(out=rstd, in0=ms, in1=nhalf[:, 0:1], op=ALU.pow),
                 r=[tag + "ms", "nhalf"], w=[tag + "rstd"])

        with contextlib.ExitStack() as p0:
            NB0, K0 = 4, 3
            gt = sb("gt0", [128, D], F32, p0)
            xt = [sb("xt0_%d" % i, [128, D], F32, p0) for i in range(NB0)]
            xb = [sb("xb0_%d" % i, [128, D], BF16, p0) for i in range(NB0)]
            junk = sb("junk0", [128, D], BF16, p0)
            st = sb("st0", [128, 4 * NB0], F32, p0)
            hTg = [sb("hTg0_%d" % i, [128, 8, 512], BF16, p0) for i in range(2)]
            s.dma("sp", lambda e: e.dma_start(out=gt[:], in_=gmix_d[0:1, :].to_broadcast([128, D])), w=["gt"])

            def load0(i):
                b = i % NB0
                s.dma("sp", lambda e: e.dma_start(out=xt[b][:], in_=x_d[i * 128:(i + 1) * 128, :]), w=["xt%d" % b])

            def stA0(i):
                b = i % NB0
                if i + K0 < NT:
                    load0(i + K0)
                sq, ms, rs = (st[:, 4 * b + k:4 * b + k + 1] for k in range(3))
                tag = "p0_%d" % b
                s.op("act", lambda e: e.activation(out=junk[:], in_=xt[b][:], func=AF.Square, accum_out=sq),
                     r=["xt%d" % b], w=["junk", tag + "ssq"])
                rstd_from_ssq(sq, ms, rs, D, tag)
                s.op("dve", lambda e: e.scalar_tensor_tensor(out=xb[b][:], in0=xt[b][:], scalar=rs, in1=gt[:],
                                                             op0=ALU.mult, op1=ALU.mult),
                     r=["xt%d" % b, tag + "rstd", "gt"], w=["xb%d" % b])

            def stB0(i):
                b = i % NB0
                g = i // 4
                pv = psb(i % 2).rearrange("p (a b) -> p a b", a=8)
                for kc in range(8):
                    s.tr(pv[:, kc, :], xb[b][:, kc * 128:(kc + 1) * 128], identb[:], r=["xb%d" % b, "identb"], w=["ps%d" % (i % 2)])
                s.op("act", lambda e: e.copy(out=hTg[g % 2][:, :, (i % 4) * 128:(i % 4 + 1) * 128], in_=pv),
                     r=["ps%d" % (i % 2)], w=["hTg%d" % (g % 2)])
                if i % 4 == 3:
                    s.dma("sp", lambda e: e.dma_start(out=hT_d[:, :, g * 512:(g + 1) * 512], in_=hTg[g % 2][:]),
                          r=["hTg%d" % (g % 2)], w=["hT_d%d" % g])

            for i in range(K0):
                load0(i)
            stA0(0)
            stA0(1)
            for i in range(NT):
                stB0(i)
                if i + 2 < NT:
                    stA0(i + 2)
        s.barrier()
        if DEBUG == "p0":
            return nc

        pw = contextlib.ExitStack()
        wuv = sb("wuv", [128, 8, 1024], BF16, pw)
        wpa = sb("wpa", [128, 4, 1024], BF16, pw)
        wpb = sb("wpb", [128, 4, 1024], BF16, pw)
        wo = sb("wo", [128, 8, 1024], BF16, pw)
        with contextlib.ExitStack() as p1:
            wq = sb("wq", [128, 3, 8, 128], BF16, p1)
            hg = [sb("hg1_%d" % i, [128, 8, 512], BF16, p1) for i in range(2)]
            qkv = sb("qkv", [128, 3, SEQ], BF16, p1)
            qT, kT, vT = qkv[:, 0, :], qkv[:, 1, :], qkv[:, 2, :]
            Vl = sb("Vl", [128, 32, 128], BF16, p1)
            PT = [sb("PT%d" % i, [128, 2, 256], BF16, p1) for i in range(4)]
            EX = [sb("EX%d" % i, [128, 2, 256], BF16, p1) for i in range(3)]
            msk = sb("msk", [128, 12, 2, 256], BF16, p1)
            accN = sb("accN", [128, SEQ], F32, p1)
            accD = sb("accD", [128, SEQ], F32, p1)
            scr1 = sb("scr1", [128, 768], F32, p1)
            dlt, band, mtmp = scr1[:, 0:256], scr1[:, 256:512], scr1[:, 512:768]
            dtmpB = sb("dtmpB", [128, 512], F32, p1)
            DT = [scr1[:, 0:512].rearrange("p (a b) -> p a b", a=2), dtmpB[:, :].rearrange("p (a b) -> p a b", a=2)]
            DTK = [["dt0", "dlt", "band"], ["dt1"]]

            s.op("pool", lambda e: e.iota(dlt, pattern=[[-1, 256]], base=64, channel_multiplier=1,
                                          allow_small_or_imprecise_dtypes=True), w=["dlt"])
            s.op("dve", lambda e: e.tensor_scalar(out=band, in0=dlt, scalar1=-1.0, scalar2=None, op0=ALU.mult), r=["dlt"], w=["band"])
            s.op("dve", lambda e: e.tensor_tensor(out=dlt, in0=dlt, in1=band, op=ALU.max), r=["dlt", "band"], w=["dlt"])
            s.op("dve", lambda e: e.tensor_scalar(out=band, in0=dlt, scalar1=64.5, scalar2=None, op0=ALU.is_le),
                 r=["dlt"], w=["band"])
            for pi, dil in enumerate(PATTERNS):
                for hp in range(4):
                    for hh in range(2):
                        h = hp * 2 + hh
                        slope = 2.0 ** (-8.0 * (h + 1) / 8.0)
                        s.op("act", lambda e: e.activation(out=mtmp, in_=dlt, func=AF.Exp, scale=-slope * dil),
                             r=["dlt"], w=["mtmp"])
                        s.op("dve", lambda e: e.tensor_tensor(out=msk[:, pi * 4 + hp, hh, :], in0=mtmp, in1=band, op=ALU.mult),
                             r=["mtmp", "band"], w=["msk"])

            ck(1)
            for hp in range(4):
                for t3 in range(3):
                    c0 = 1024 + 512 * t3 + hp * 128
                    s.dma("pool", lambda e: e.dma_start(out=wq[:, t3, :, :],
                                                        in_=win_d[:, c0:c0 + 128].rearrange("(kc p) c -> p kc c", p=128)),
                          w=["wq"])
                if hp == 0:
                    castw = lambda dst, src, key: s.dma("pool", lambda e: e.dma_start(out=dst, in_=src), w=[key])
                    castw(wuv[:], win_d[:, 0:1024].rearrange("(kc p) c -> p kc c", p=128), "wuv")
                    castw(wpa[:], wpa_d.rearrange("(kc p) c -> p kc c", p=128), "wpa")
                    castw(wpb[:], wpb_d.rearrange("(kc p) c -> p kc c", p=128), "wpb")
                    castw(wo[:], wout_d.rearrange("(kc p) c -> p kc c", p=128), "wo")
                for g in range(8):
                    hb = g % 2
                    s.dma("sp", lambda e: e.dma_start(out=hg[hb][:], in_=hT_d[:, :, g * 512:(g + 1) * 512]),
                          r=["hT_d"], w=["hg%d" % hb])
                    for t3 in range(3):
                        bank = (g * 3 + t3) % 2
                        for kc in range(8):
                            s.mm(PS[bank][:, :], lhsT=wq[:, t3, kc, :], rhs=hg[hb][:, kc, :], start=(kc == 0), stop=(kc == 7),
                                 r=["wq", "hg%d" % hb], w=["ps%d" % bank])
                        s.op("act", lambda e: e.copy(out=qkv[:, t3, g * 512:(g + 1) * 512], in_=PS[bank][:, :]),
                             r=["ps%d" % bank], w=["qkv%d" % t3])
                ck(2)
                s.op("pool", lambda e: e.memset(accN[:], 0.0), w=["accN"])
                s.op("pool", lambda e: e.memset(accD[:], 0.0), w=["accD"])
                blk_ctr = 0
                grp_ctr = 0
                pend = []

                def flushB(keep):
                    while len(pend) > keep:
                        pend.pop(0)()

                def mkA(pi, dil, r, j, L, bc):
                    def fn():
                        qa = max(0, 128 * j - 64)
                        qb_ = min(L, 128 * j + 192)
                        c0 = qa - (128 * j - 64)
                        n = qb_ - qa
                        sw = bc % 2
                        spv = PSW[sw][:, :].rearrange("p (a b) -> p a b", a=2)
                        skeys = ["ps%d" % (2 * sw), "ps%d" % (2 * sw + 1)]
                        for hh in range(2):
                            lo, hi = hh * 64, hh * 64 + 64
                            s.mm(spv[:, hh, c0:c0 + n], lhsT=tokv(kT[lo:hi], r, dil, j * 128, 128),
                                 rhs=tokv(qT[lo:hi], r, dil, qa, n), r=["qkv0", "qkv1"], w=[skeys[hh]])
                        ex = EX[bc % 3]
                        pt = PT[bc % 4]
                        s.op("act", lambda e: e.activation(out=ex[:, :, c0:c0 + n], in_=spv[:, :, c0:c0 + n], func=AF.Exp, scale=0.125),
                             r=skeys, w=["EX%d" % (bc % 3)])
                        s.op("dve", lambda e: e.tensor_tensor(out=pt[:, :, c0:c0 + n], in0=ex[:, :, c0:c0 + n],
                                                               in1=msk[:, pi * 4 + hp, :, c0:c0 + n], op=ALU.mult),
                             r=["EX%d" % (bc % 3), "msk"], w=["PT%d" % (bc % 4)])
                    return fn

                def mkB(pi, dil, r, i, L, nb, sl, gcn):
                    def fn():
                        ma = max(0, 128 * i - 64)
                        mb = min(L, 128 * i + 64)
                        n = mb - ma
                        js = [jj for jj in (i - 1, i) if 0 <= jj < nb]
                        nbank = 4 + gcn % 2
                        dbank = 6 + gcn % 2
                        nv = PS[nbank][:, :].rearrange("p (a b) -> p a b", a=2)
                        dv = PS[dbank][:, :].rearrange("p (a b) -> p a b", a=2)
                        col = (i % 2) * 128 + (ma - (128 * i - 64))
                        for ji, jj in enumerate(js):
                            c = ma - (128 * jj - 64)
                            ptj = PT[sl[jj]]
                            s.mm(nv[:, :, col:col + n], lhsT=Vl[:, r * nb + jj, :], rhs=ptj[:, :, c:c + n],
                                 start=(ji == 0), stop=(ji == len(js) - 1),
                                 r=["Vl", "PT%d" % sl[jj]], w=["ps%d" % nbank])
                        for ji, jj in enumerate(js):
                            c = ma - (128 * jj - 64)
                            ptj = PT[sl[jj]]
                            s.mm(dv[:, :, col:col + n], lhsT=onesb[:], rhs=ptj[:, :, c:c + n],
                                 start=(ji == 0), stop=(ji == len(js) - 1),
                                 r=["onesb", "PT%d" % sl[jj]], w=["ps%d" % dbank])
                        if i % 2 == 1 or i == nb:
                            g = i // 2
                            ga = max(0, 256 * g - 64)
                            gb_ = min(L, 256 * g + 192)
                            gn = gb_ - ga
                            gc = ga - (256 * g - 64)
                            dtv = DT[gcn % 2]
                            dtk = DTK[gcn % 2]
                            s.op("act", lambda e: e.copy(out=dtv[:, :, gc:gc + gn], in_=dv[:, :, gc:gc + gn]),
                                 r=["ps%d" % dbank], w=dtk)
                            for hh in range(2):
                                lo, hi = hh * 64, hh * 64 + 64
                                av = tokv(accN[lo:hi], r, dil, ga, gn)
                                s.op("dve", lambda e: e.tensor_tensor(out=av, in0=av, in1=nv[lo:hi, hh, gc:gc + gn], op=ALU.add),
                                     r=["ps%d" % nbank, "accN"], w=["accN"])
                                ad = tokv(accD[lo:hi], r, dil, ga, gn)
                                s.op("pool", lambda e: e.tensor_tensor(out=ad, in0=ad, in1=dtv[lo:hi, hh, gc:gc + gn], op=ALU.add),
                                     r=[dtk[0], "accD"], w=["accD"])
                    return fn

                for pi, dil in enumerate(PATTERNS):
                    L = SEQ // dil
                    nb = L // 128
                    flushB(0)
                    for r in range(dil):
                        for j in range(nb):
                            blk = r * nb + j
                            pvw = psb(2).rearrange("p (a b) -> p a b", a=8)
                            s.tr(pvw[:, blk % 8, :], tokv(vT, r, dil, j * 128, 128), identb[:], r=["qkv2", "identb"], w=["ps2"])
                            if blk % 8 == 7:
                                s.op("act", lambda e: e.copy(out=Vl[:, blk - 7:blk + 1, :], in_=pvw), r=["ps2"], w=["Vl"])
                    for r in range(dil):
                        slots = {}
                        for j in range(nb + 1):
                            if j < nb:
                                mkA(pi, dil, r, j, L, blk_ctr)()
                                slots[j] = blk_ctr % 4
                                blk_ctr += 1
                            pend.append(mkB(pi, dil, r, j, L, nb, dict(slots), grp_ctr))
                            if j % 2 == 1 or j == nb:
                                grp_ctr += 1
                            flushB(SKEW)
                flushB(0)
                for hf in range(2):
                    sl = slice(hf * 2048, (hf + 1) * 2048)
                    s.op("dve", lambda e: e.reciprocal(out=accD[:, sl], in_=accD[:, sl]), r=["accD"], w=["accD"])
                    s.op("dve", lambda e: e.tensor_tensor(out=oT[:, hp, sl], in0=accN[:, sl], in1=accD[:, sl], op=ALU.mult),
                         r=["accN", "accD"], w=["oT"])
        s.barrier()
        if DEBUG == "oT":
            with contextlib.ExitStack() as pd:
                tmpf = sb("dbgtmp", [128, SEQ], F32, pd)
                for hp in range(4):
                    s.op("dve", lambda e: e.tensor_copy(out=tmpf[:], in_=oT[:, hp, :]), r=["oT"], w=["tmpf"])
                    s.dma("sp", lambda e: e.dma_start(out=dbg_d[:, hp * SEQ:(hp + 1) * SEQ], in_=tmpf[:]), r=["tmpf"], w=["dbg"])
                s.barrier()
            return nc

        with contextlib.ExitStack() as p2:
            wgg = sb("wgg", [128, 8, 2048], BF16, p2)
            wrt = sb("wrt", [128, 8, NE], BF16, p2)
            wsf = sb("wsf", [128, 8, 128], F32, p2)
            wsT = sb("wsT", [128, 8, 128], BF16, p2)
            bspr = sb("bspr", [8, 128], F32, p2)
            bspt = sb("bspt", [128, 8], F32, p2)
            bg = sb("bg", [128, 16], F32, p2)
            bgr = sb("bgr", [16, 128], F32, p2)
            ggt = sb("ggt", [128, 512], F32, p2)
            g2t = sb("g2t", [128, D], F32, p2)
            hg = [sb("hg2_%d" % i, [128, 8, 512], BF16, p2) for i in range(2)]
            gu = sb("gu", [128, 512], BF16, p2)
            gv = sb("gv", [128, 512], F32, p2)
            vn = sb("vn", [128, 512], BF16, p2)
            ab = sb("ab", [128, 512], BF16, p2)
            aT = [sb("aT%d" % i, [128, 4, 512], BF16, p2) for i in range(2)]
            sga = sb("sga", [128, 512], F32, p2)
            sgb = sb("sgb", [128, 512], F32, p2)
            t1 = sga
            t2 = sgb
            mT = [sb("mT%d" % i, [128, 8, 512], BF16, p2) for i in range(2)]
            x1 = [sb("x1_%d" % i, [128, D], F32, p2) for i in range(2)]
            hrow = [sb("hrow%d" % i, [128, ROW], BF16, p2) for i in range(2)]
            h2T = sb("h2T", [128, 8, 128], BF16, p2)
            st = sb("st2", [128, 16], F32, p2)
            lg = sb("lg", [128, NE], F32, p2)
            aff = sb("aff", [128, NE], F32, p2)

            cast = lambda dst, src, key: s.dma("pool", lambda e: e.dma_start(out=dst, in_=src), w=[key])
            s.dma("sp", lambda e: e.dma_start(out=wsf[:], in_=wsp_d.rearrange("g t s -> t g s")), w=["wsf"])
            s.dma("sp", lambda e: e.dma_start(out=bspr[:], in_=bsp_d), w=["bspr"])
            s.dma("sp", lambda e: e.dma_start(out=bgr[:], in_=bgate_d), w=["bgr"])
            s.dma("sp", lambda e: e.dma_start(out=ggt[:], in_=ggm_d[0:1, :].to_broadcast([128, 512])), w=["ggt"])
            s.dma("sp", lambda e: e.dma_start(out=g2t[:], in_=gffn_d[0:1, :].to_broadcast([128, D])), w=["g2t"])
            cast(wgg[:], win_d[:, 2560:4608].rearrange("(kc p) c -> p kc c", p=128), "wgg")
            cast(wrt[:], wr_d.rearrange("(kc p) c -> p kc c", p=128), "wrt")
            for g in range(8):
                s.tr(PS[0][:, 0:128], wsf[:, g, :], identf[:], r=["wsf", "identf"], w=["ps0"])
                s.op("dve", lambda e: e.tensor_copy(out=wsT[:, g, :], in_=PS[0][:, 0:128]), r=["ps0"], w=["wsT"])
            s.tr(PS[0][:, 0:8], bspr[:, :], identf[0:8, 0:8], r=["bspr", "identf"], w=["ps0"])
            s.op("dve", lambda e: e.tensor_copy(out=bspt[:], in_=PS[0][:, 0:8]), r=["ps0"], w=["bspt"])
            s.tr(PS[0][:, 0:16], bgr[:, :], identf[0:16, 0:16], r=["bgr", "identf"], w=["ps0"])
            s.op("dve", lambda e: e.tensor_copy(out=bg[:], in_=PS[0][:, 0:16]), r=["ps0"], w=["bg"])

            def Ia(g, t):
                hb = g % 2
                tsl = slice(t * 128, (t + 1) * 128)
                if t == 0:
                    s.dma("act", lambda e: e.dma_start(out=hg[hb][:], in_=hT_d[:, :, g * 512:(g + 1) * 512]), r=["hT_d"], w=["hg%d" % hb])
                for half in range(2):
                    for kc in range(8):
                        s.mm(PS[half][:, :], lhsT=hg[hb][:, kc, tsl], rhs=wuv[:, kc, half * 512:(half + 1) * 512],
                             start=(kc == 0), stop=(kc == 7), r=["hg%d" % hb, "wuv"], w=["ps%d" % half])
                s.op("act", lambda e: e.activation(out=gu[:], in_=PS[0][:, :], func=AF.Gelu_apprx_tanh), r=["ps0"], w=["gu"])
                s.op("act", lambda e: e.activation(out=gv[:], in_=PS[1][:, :], func=AF.Gelu_apprx_tanh), r=["ps1"], w=["gv"])
                s.op("act", lambda e: e.activation(out=vn[:], in_=gv[:], func=AF.Square, accum_out=st[:, 0:1]),
                     r=["gv"], w=["vn", "gmssq"])
                rstd_from_ssq(st[:, 0:1], st[:, 1:2], st[:, 2:3], 512, "gm")
                s.op("dve", lambda e: e.scalar_tensor_tensor(out=vn[:], in0=gv[:], scalar=st[:, 2:3], in1=ggt[:],
                                                             op0=ALU.mult, op1=ALU.mult),
                     r=["gv", "gmrstd", "ggt"], w=["vn"])

            def Ib(g, t):
                for gg in range(8):
                    s.mm(PS[2][:, gg * 64:(gg + 1) * 64], lhsT=wsT[:, gg, :], rhs=vn[:, gg * 64:(gg + 1) * 64],
                         r=["wsT", "vn"], w=["ps2"])
                s.op("dve", lambda e: e.tensor_tensor(out=gv[:].rearrange("p (a b) -> p a b", a=8),
                                                      in0=PS[2][:, :].rearrange("p (a b) -> p a b", a=8),
                                                      in1=bspt[:].unsqueeze(2).to_broadcast([128, 8, 64]), op=ALU.add),
                     r=["ps2", "bspt"], w=["gv"])
                s.op("dve", lambda e: e.tensor_tensor(out=ab[:], in0=gv[:], in1=gu[:], op=ALU.mult), r=["gv", "gu"], w=["ab"])

            def Ic(g, t):
                tsl = slice(t * 128, (t + 1) * 128)
                pv = psb(3).rearrange("p (a b) -> p a b", a=8)
                for kc in range(4):
                    s.tr(pv[:, kc, :], ab[:, kc * 128:(kc + 1) * 128], identb[:], r=["ab", "identb"], w=["ps3"])
                s.op("act", lambda e: e.copy(out=aT[g % 2][:, :, tsl], in_=pv[:, 0:4, :]), r=["ps3"], w=["aT%d" % (g % 2)])

            def II(g, c):
                hb = g % 2
                gsl = slice(g * 512, (g + 1) * 512)
                csl = slice(c * 128, (c + 1) * 128)
                aTg = aT[g % 2]
                for kc in range(8):
                    s.mm(PS[4][:, :], lhsT=wgg[:, kc, csl], rhs=hg[hb][:, kc, :], start=(kc == 0), stop=(kc == 7),
                         r=["wgg", "hg%d" % hb], w=["ps4"])
                for kc in range(8):
                    s.mm(PS[5][:, :], lhsT=wgg[:, kc, 1024 + c * 128:1024 + (c + 1) * 128], rhs=hg[hb][:, kc, :],
                         start=(kc == 0), stop=(kc == 7), r=["wgg", "hg%d" % hb], w=["ps5"])
                for kc in range(4):
                    s.mm(PS[6][:, :], lhsT=wpa[:, kc, csl], rhs=aTg[:, kc, :], start=(kc == 0), stop=(kc == 3),
                         r=["wpa", "aT%d" % (g % 2)], w=["ps6"])
                for kc in range(4):
                    s.mm(PS[7][:, :], lhsT=wpb[:, kc, csl], rhs=oT[:, kc, gsl], start=(kc == 0), stop=(kc == 3),
                         r=["wpb", "oT"], w=["ps7"])
                s.op("act", lambda e: e.activation(out=sga[:], in_=PS[4][:, :], func=AF.Sigmoid, bias=bg[:, c:c + 1], scale=1.0),
                     r=["ps4", "bg"], w=["sga"])
                s.op("act", lambda e: e.activation(out=sgb[:], in_=PS[5][:, :], func=AF.Sigmoid, bias=bg[:, 8 + c:9 + c], scale=1.0),
                     r=["ps5", "bg"], w=["sgb"])
                s.op("dve", lambda e: e.tensor_tensor(out=t1[:], in0=PS[6][:, :], in1=sga[:], op=ALU.mult), r=["ps6", "sga"], w=["sga"])
                s.op("dve", lambda e: e.tensor_tensor(out=t2[:], in0=PS[7][:, :], in1=sgb[:], op=ALU.mult), r=["ps7", "sgb"], w=["sgb"])
                s.op("pool", lambda e: e.tensor_tensor(out=mT[g % 2][:, c, :], in0=t1[:], in1=t2[:], op=ALU.add),
                     r=["sga", "sgb"], w=["mT%d" % (g % 2)])

            def IIIa(g, t):
                i = g * 4 + t
                b = i % 2
                tsl = slice(t * 128, (t + 1) * 128)
                s.dma("act", lambda e: e.dma_start(out=x1[b][:], in_=x_d[i * 128:(i + 1) * 128, :]), w=["x1_%d" % b])
                for half in range(2):
                    for kc in range(8):
                        s.mm(PS[half][:, :], lhsT=mT[g % 2][:, kc, tsl], rhs=wo[:, kc, half * 512:(half + 1) * 512],
                             start=(kc == 0), stop=(kc == 7), r=["mT%d" % (g % 2), "wo"], w=["ps%d" % half])
                    hs = slice(half * 512, (half + 1) * 512)
                    s.op("dve", lambda e: e.tensor_tensor(out=x1[b][:, hs], in0=PS[half][:, :], in1=x1[b][:, hs], op=ALU.add),
                         r=["ps%d" % half, "x1_%d" % b], w=["x1_%d" % b])
                s.dma("sp", lambda e: e.dma_start(out=x1_d[i * 128:(i + 1) * 128, :], in_=x1[b][:]), r=["x1_%d" % b], w=["x1_d%d" % i])
                s.op("act", lambda e: e.activation(out=hrow[b][:, 0:D], in_=x1[b][:], func=AF.Square, accum_out=st[:, 4:5]),
                     r=["x1_%d" % b], w=["hrow%d" % b, "rtssq"])
                rstd_from_ssq(st[:, 4:5], st[:, 5:6], st[:, 6:7], D, "rt")
                s.op("dve", lambda e: e.scalar_tensor_tensor(out=hrow[b][:, 0:D], in0=x1[b][:], scalar=st[:, 6:7], in1=g2t[:],
                                                             op0=ALU.mult, op1=ALU.mult),
                     r=["x1_%d" % b, "rtrstd", "g2t"], w=["hrow%d" % b])

            def IIIb1(g, t):
                i = g * 4 + t
                b = i % 2
                pv = psb(3).rearrange("p (a b) -> p a b", a=8)
                for kc in range(8):
                    s.tr(pv[:, kc, :], hrow[b][:, kc * 128:(kc + 1) * 128], identb[:], r=["hrow%d" % b, "identb"], w=["ps3"])
                s.op("act", lambda e: e.copy(out=h2T[:], in_=pv), r=["ps3"], w=["h2T"])
                s.dma("sp", lambda e: e.dma_start(out=h2_d[i * 128:(i + 1) * 128, :], in_=hrow[b][:]), r=["hrow%d" % b], w=["h2_d%d" % i])

            def IIIb2(g, t):
                i = g * 4 + t
                for kc in range(8):
                    s.mm(PS[2][:, 0:NE], lhsT=h2T[:, kc, :], rhs=wrt[:, kc, :], start=(kc == 0), stop=(kc == 7),
                         r=["h2T", "wrt"], w=["ps2"])
                s.op("dve", lambda e: e.tensor_reduce(out=st[:, 8:9], in_=PS[2][:, 0:NE], axis=AX.X, op=ALU.max), r=["ps2"], w=["smx"])
                s.op("dve", lambda e: e.tensor_scalar(out=st[:, 9:10], in0=st[:, 8:9], scalar1=-1.0, scalar2=None, op0=ALU.mult),
                     r=["smx"], w=["snmx"])
                s.op("act", lambda e: e.activation(out=lg[:], in_=PS[2][:, 0:NE], func=AF.Exp, bias=st[:, 9:10], scale=1.0,
                                                   accum_out=st[:, 10:11]),
                     r=["ps2", "snmx"], w=["lg", "ssum"])
                s.op("dve", lambda e: e.reciprocal(out=st[:, 11:12], in_=st[:, 10:11]), r=["ssum"], w=["srs"])
                s.op("dve", lambda e: e.tensor_scalar(out=aff[:], in0=lg[:], scalar1=st[:, 11:12], scalar2=None, op0=ALU.mult),
                     r=["lg", "srs"], w=["aff"])
                s.dma("sp", lambda e: e.dma_start(out=aff_d[i * 128:(i + 1) * 128, :], in_=aff[:]), r=["aff"], w=["aff_d%d" % i])

            def IIIc(g, t):
                i = g * 4 + t
                s.tr(PS[2][0:16, 128:256], aff[:, :], identf[:], r=["aff", "identf"], w=["ps2"])
                s.op("act", lambda e: e.copy(out=affT[:, i * 128:(i + 1) * 128], in_=PS[2][0:16, 128:256]), r=["ps2"], w=["affT"])

            NSLOT = 8 * 10 + 12
            pre = [[] for _ in range(NSLOT)]
            mid = [[] for _ in range(NSLOT)]
            post = [[] for _ in range(NSLOT)]
            last = [[] for _ in range(NSLOT)]
            mk = lambda f, g, t: (lambda: f(g, t))
            for g in range(8):
                for t in range(4):
                    b0 = 8 * g
                    last[b0 + 2 * t].append(mk(Ia, g, t))
                    post[b0 + 2 * t + 1].append(mk(Ib, g, t))
                    pre[b0 + 2 * t + 2].append(mk(Ic, g, t))
                    b2 = 8 * (g + 2)
                    last[b2 + 2 * t + 1].append(mk(IIIa, g, t))
                    post[b2 + 2 * t + 2].append(mk(IIIb1, g, t))
                    pre[b2 + 2 * t + 3].append(mk(IIIb2, g, t))
                    post[b2 + 2 * t + 3].append(mk(IIIc, g, t))
                for c in range(8):
                    mid[8 * (g + 1) + c].append(mk(II, g, c))
            for sl in range(NSLOT):
                for lst in (pre[sl], mid[sl], post[sl], last[sl]):
                    for fn in lst:
                        fn()
        pw.close()
        st_oT.close()
        s.barrier()

        with contextlib.ExitStack() as p3:
            cmp_ = sb("cmp", [16, SEQ], F32, p3)
            ones16 = sb("ones16", [16, SEQ], F32, p3)
            cs = sb("cs", [16, SEQ], F32, p3)
            bs = sb("bs", [16, 8], F32, p3)
            csT = sb("csT", [128, NT * NE], F32, p3)
            csI = sb("csI", [128, NT * NE], I32, p3)
            aI = sb("aI", [128, NT * NE], I32, p3)
            bI = sb("bI", [128, NT * NE], I32, p3)
            aF = sb("aF", [128, NT, NE], BF16, p3)
            bF = sb("bF", [128, NT, NE], F32, p3)
            io4 = sb("io4", [128, 4], F32, p3)
            ones4 = sb("ones4", [128, NE, 4], BF16, p3)
            EQ = [sb("EQ%d" % i, [128, NE, 128], BF16, p3) for i in range(2)]
            LT = [sb("LT%d" % i, [128, NE, 128], BF16, p3) for i in range(2)]
            LEb = [sb("LEb%d" % i, [128, NE, 4], BF16, p3) for i in range(2)]
            idxf = sb("idxf", [128, NE * 4], F32, p3)
            lo, hi, mid, cnt, ge, dd = (bs[:, k:k + 1] for k in range(6))
            s.op("dve", lambda e: e.memset(bs[:, 0:1], 0.0), w=["lo"])
            s.op("dve", lambda e: e.memset(bs[:, 1:2], 1.0), w=["hi"])
            s.op("pool", lambda e: e.memset(ones16[:], 1.0), w=["ones16"])
            s.op("pool", lambda e: e.memset(ones4[:], 1.0), w=["ones4"])
            s.op("pool", lambda e: e.iota(io4[:], pattern=[[1, 4]], base=0, channel_multiplier=0,
                                          allow_small_or_imprecise_dtypes=True), w=["io4"])
            for it in range(30):
                s.op("dve", lambda e: e.tensor_tensor(out=mid, in0=lo, in1=hi, op=ALU.add), r=["lo", "hi"], w=["mid"])
                s.op("dve", lambda e: e.tensor_scalar(out=mid, in0=mid, scalar1=0.5, scalar2=None, op0=ALU.mult), r=["mid"], w=["mid"])
                s.op("dve", lambda e: e.tensor_scalar(out=cmp_[:], in0=affT[:], scalar1=mid, scalar2=0.0, op0=ALU.is_ge, op1=ALU.add,
                                                      accum_out=cnt),
                     r=["affT", "mid"], w=["cmp", "cnt"])
                s.op("dve", lambda e: e.tensor_scalar(out=ge, in0=cnt, scalar1=CAP - 0.5, scalar2=None, op0=ALU.is_ge), r=["cnt"], w=["ge"])
                s.op("dve", lambda e: e.tensor_tensor(out=dd, in0=mid, in1=lo, op=ALU.subtract), r=["mid", "lo"], w=["dd"])
                s.op("dve", lambda e: e.scalar_tensor_tensor(out=lo, in0=dd, scalar=ge, in1=lo, op0=ALU.mult, op1=ALU.add),
                     r=["dd", "ge", "lo"], w=["lo"])
                s.op("dve", lambda e: e.tensor_tensor(out=dd, in0=hi, in1=mid, op=ALU.subtract), r=["mid", "hi"], w=["dd"])
                s.op("dve", lambda e: e.scalar_tensor_tensor(out=hi, in0=dd, scalar=ge, in1=mid, op0=ALU.mult, op1=ALU.add),
                     r=["dd", "ge", "mid"], w=["hi"])
            s.op("dve", lambda e: e.tensor_scalar(out=cmp_[:], in0=affT[:], scalar1=lo, scalar2=None, op0=ALU.is_ge), r=["affT", "lo"], w=["cmp"])
            s.op("dve", lambda e: e.tensor_tensor_scan(out=cs[:], data0=ones16[:], data1=cmp_[:], initial=0.0, op0=ALU.mult, op1=ALU.add),
                 r=["cmp", "ones16"], w=["cs"])
            for i in range(NT):
                s.tr(PS[0][:, i * NE:(i + 1) * NE], cs[:, i * 128:(i + 1) * 128], identf[0:16, 0:16], r=["cs", "identf"], w=["ps0"])
            s.op("dve", lambda e: e.tensor_copy(out=csT[:], in_=PS[0][:, :]), r=["ps0"], w=["csT"])
            s.op("dve", lambda e: e.tensor_copy(out=csI[:], in_=csT[:]), r=["csT"], w=["csI"])
            s.op("dve", lambda e: e.tensor_single_scalar(out=aI[:], in_=csI[:], scalar=2, op=ALU.arith_shift_right), r=["csI"], w=["aI"])
            s.op("dve", lambda e: e.tensor_single_scalar(out=bI[:], in_=csI[:], scalar=3, op=ALU.bitwise_and), r=["csI"], w=["bI"])
            s.op("dve", lambda e: e.tensor_copy(out=aF[:].rearrange("p a b -> p (a b)"), in_=aI[:]), r=["aI"], w=["aF"])
            s.op("dve", lambda e: e.tensor_copy(out=bF[:].rearrange("p a b -> p (a b)"), in_=bI[:]), r=["bI"], w=["bF"])
            ioc = sb("ioc", [128, 128], F32, p3)
            s.op("pool", lambda e: e.iota(ioc[:], pattern=[[1, 128]], base=0, channel_multiplier=0,
                                          allow_small_or_imprecise_dtypes=True), w=["ioc"])
            iocb = sb("iocb", [128, 128], BF16, p3)
            s.op("dve", lambda e: e.tensor_copy(out=iocb[:], in_=ioc[:]), r=["ioc"], w=["iocb"])
            idv = PS[1][:, 0:NE * 4].rearrange("p (a b) -> p a b", a=NE)
            for i in range(NT):
                b = i % 2
                s.op("dve", lambda e: e.tensor_tensor(out=EQ[b][:], in0=aF[:, i, :].unsqueeze(2).to_broadcast([128, NE, 128]),
                                                      in1=iocb[:].unsqueeze(1).to_broadcast([128, NE, 128]), op=ALU.is_equal),
                     r=["aF", "iocb"], w=["EQ%d" % b])
                s.op("dve", lambda e: e.tensor_tensor(out=LT[b][:], in0=aF[:, i, :].unsqueeze(2).to_broadcast([128, NE, 128]),
                                                       in1=iocb[:].unsqueeze(1).to_broadcast([128, NE, 128]), op=ALU.is_lt),
                     r=["aF", "iocb"], w=["LT%d" % b])
                s.op("dve", lambda e: e.tensor_tensor(out=LEb[b][:], in0=bF[:, i, :].unsqueeze(2).to_broadcast([128, NE, 4]),
                                                      in1=io4[:].unsqueeze(1).to_broadcast([128, NE, 4]), op=ALU.is_le),
                     r=["bF", "io4"], w=["LEb%d" % b])
                for ex in range(NE):
                    s.mm(idv[:, ex, :], lhsT=EQ[b][:, ex, :], rhs=LEb[b][:, ex, :], start=(i == 0 and ex == 0), stop=False,
                         r=["EQ%d" % b, "LEb%d" % b], w=["ps1"], skip=True)
                    s.mm(idv[:, ex, :], lhsT=LT[b][:, ex, :], rhs=ones4[:, ex, :], start=False, stop=(i == NT - 1),
                         r=["LT%d" % b, "ones4"], w=["ps1"], skip=True)
            s.op("dve", lambda e: e.tensor_copy(out=idxf[:], in_=PS[1][:, 0:NE * 4]), r=["ps1"], w=["idxf"])
            s.op("dve", lambda e: e.tensor_copy(out=idxi[:].rearrange("p a b -> p (a b)"), in_=idxf[:]), r=["idxf"], w=["idxi"])
        s.barrier()
        if DEBUG == "idx":
            with contextlib.ExitStack() as pd:
                tmpf = sb("dbgtmp", [128, SEQ], F32, pd)
                s.op("pool", lambda e: e.memset(tmpf[:], 0.0), w=["tmpf"])
                s.op("dve", lambda e: e.tensor_copy(out=tmpf[:, 0:64], in_=idxi[:].rearrange("p a b -> p (a b)")), r=["idxi"], w=["tmpf"])
                s.dma("sp", lambda e: e.dma_start(out=dbg_d[:, 0:SEQ], in_=tmpf[:]), r=["tmpf"], w=["dbg"])
                s.op("dve", lambda e: e.tensor_copy(out=tmpf[0:16, :], in_=affT[:]), r=["affT"], w=["tmpf"])
                s.dma("sp", lambda e: e.dma_start(out=dbg_d[:, SEQ:2 * SEQ], in_=tmpf[:]), r=["tmpf"], w=["dbg"])
                s.barrier()
            return nc

        st_aff.close()
        with contextlib.ExitStack() as p4:
            wg = [sb("wg%d" % i, [128, 8, D], BF16, p4) for i in range(2)]
            wu = [sb("wu%d" % i, [128, 8, D], BF16, p4) for i in range(2)]
            wd = [sb("wd%d" % i, [128, 8, D], BF16, p4) for i in range(2)]
            Xe = [sb("Xe%d" % i, [128, 4, ROW], BF16, p4) for i in range(2)]
            XeT = sb("XeT", [128, 8, CAP], BF16, p4)
            hid = sb("hid", [128, 8, CAP], BF16, p4)
            sg = sb("sg", [128, CAP], F32, p4)
            ye = [sb("ye%d" % i, [128, D], F32, p4) for i in range(2)]
            Ge = [sb("Ge%d" % i, [128, 4, NE], F32, p4) for i in range(2)]

            NSTG = 6
            stg = [sb("stg%d" % i, [128, 2, D], F32, p4) for i in range(NSTG)]
            chunks = [(ex_, mi, q) for ex_ in range(NE) for mi in range(3) for q in range(4)]
            mats = ((wg, weg_d, "wg"), (wu, weu_d, "wu"), (wd, wed_d, "wd"))

            def emit_dma(n):
                ex_, mi, q = chunks[n]
                k = n % NSTG
                src = mats[mi][1]
                s.dma("sp", lambda e: e.dma_start(out=stg[k][:], in_=src[ex_, q * 256:(q + 1) * 256, :].rearrange("(kc p) c -> p kc c", p=128)),
                      w=["stg%d" % k])

            def emit_cast(n):
                ex_, mi, q = chunks[n]
                k = n % NSTG
                b_ = ex_ % 2
                dst = mats[mi][0][b_]
                wkey = "%s%d_%d" % (mats[mi][2], b_, q)
                if n % 2 == 0:
                    s.op("act", lambda e: e.copy(out=dst[:, 2 * q:2 * q + 2, :], in_=stg[k][:]), r=["stg%d" % k], w=[wkey])
                else:
                    s.op("dve", lambda e: e.tensor_copy(out=dst[:, 2 * q:2 * q + 2, :], in_=stg[k][:]), r=["stg%d" % k], w=[wkey])
                if n + NSTG < len(chunks):
                    emit_dma(n + NSTG)

            def load_w(ex_):
                return [(lambda n=n: emit_cast(n)) for n in range(12 * ex_, 12 * ex_ + 12)]

            for n in range(NSTG):
                emit_dma(n)

            def gather(ex):
                b = ex % 2
                for c0 in range(4):
                    s.dma("pool", lambda e: e.indirect_dma_start(out=Xe[b][:, c0, :], out_offset=None, in_=h2_d[:, :],
                                                                 in_offset=bass.IndirectOffsetOnAxis(ap=idxi[:, ex, c0:c0 + 1], axis=0)),
                          r=["idxi", "h2_d"], w=["Xe%d_%d" % (b, c0)])
                    s.dma("pool", lambda e: e.indirect_dma_start(out=Ge[b][:, c0, :], out_offset=None, in_=aff_d[:, :],
                                                                 in_offset=bass.IndirectOffsetOnAxis(ap=idxi[:, ex, c0:c0 + 1], axis=0)),
                          r=["idxi", "aff_d"], w=["Ge%d_%d" % (b, c0)])

            for fn in load_w(0):
                fn()
            gather(0)
            yctr = 0
            for ex in range(NE):
                b = ex % 2
                casts = []
                if ex + 1 < NE:
                    casts = load_w(ex + 1)
                    gather(ex + 1)
                for c0 in range(4):
                    pv = psb(c0 % 2).rearrange("p (a b) -> p a b", a=8)
                    for kc in range(8):
                        s.tr(pv[:, kc, :], Xe[b][:, c0, kc * 128:(kc + 1) * 128], identb[:],
                             r=["Xe%d_%d" % (b, c0), "identb"], w=["ps%d" % (c0 % 2)])
                    s.op("act", lambda e: e.copy(out=XeT[:, :, c0 * 128:(c0 + 1) * 128], in_=pv), r=["ps%d" % (c0 % 2)], w=["XeT"])
                for fc in range(8):
                    fsl = slice(fc * 128, (fc + 1) * 128)
                    gb_ = 2 + (fc % 2) * 2
                    for kc in range(8):
                        s.mm(PS[gb_][:, :], lhsT=wg[b][:, kc, fsl], rhs=XeT[:, kc, :], start=(kc == 0), stop=(kc == 7),
                             r=["wg%d_%d" % (b, kc // 2), "XeT"], w=["ps%d" % gb_])
                    for kc in range(8):
                        s.mm(PS[gb_ + 1][:, :], lhsT=wu[b][:, kc, fsl], rhs=XeT[:, kc, :], start=(kc == 0), stop=(kc == 7),
                             r=["wu%d_%d" % (b, kc // 2), "XeT"], w=["ps%d" % (gb_ + 1)])
                    s.op("act", lambda e: e.activation(out=sg[:], in_=PS[gb_][:, :], func=AF.Silu), r=["ps%d" % gb_], w=["sg"])
                    s.op("dve", lambda e: e.tensor_tensor(out=hid[:, fc, :], in0=PS[gb_ + 1][:, :], in1=sg[:], op=ALU.mult),
                         r=["ps%d" % (gb_ + 1), "sg"], w=["hid"])
                    if casts:
                        casts.pop(0)()
                for c0 in range(4):
                    yb = yctr % 2
                    yctr += 1
                    gatev = Ge[b][:, c0, :]
                    for half in range(2):
                        bank = 6 + half
                        for fc in range(8):
                            s.mm(PS[bank][:, :], lhsT=hid[:, fc, c0 * 128:(c0 + 1) * 128], rhs=wd[b][:, fc, half * 512:(half + 1) * 512],
                                 start=(fc == 0), stop=(fc == 7), r=["hid", "wd%d_%d" % (b, fc // 2)], w=["ps%d" % bank])
                        s.op("dve", lambda e: e.tensor_scalar(out=ye[yb][:, half * 512:(half + 1) * 512], in0=PS[bank][:, :],
                                                              scalar1=gatev[:, ex:ex + 1], scalar2=None, op0=ALU.mult),
                             r=["ps%d" % bank, "Ge%d_%d" % (b, c0)], w=["ye%d" % yb])
                    s.dma("pool", lambda e: e.indirect_dma_start(out=x1_d[:, :], out_offset=bass.IndirectOffsetOnAxis(ap=idxi[:, ex, c0:c0 + 1], axis=0),
                                                                 in_=ye[yb][:, :], in_offset=None, compute_op=ALU.add),
                          r=["ye%d" % yb, "idxi"] + ["sc%d_%d" % (ex - 1, k) for k in range(4)], w=["sc%d_%d" % (ex, c0)])
                    if casts:
                        casts.pop(0)()
                while casts:
                    casts.pop(0)()
        s.barrier()

        with contextlib.ExitStack() as p5:
            NB5, K5 = 6, 4
            gt = sb("gt5", [128, D], F32, p5)
            xt = [sb("xt5_%d" % i, [128, D], F32, p5) for i in range(NB5)]
            ot = [sb("ot5_%d" % i, [128, D], F32, p5) for i in range(NB5)]
            junk = sb("junk5", [128, D], BF16, p5)
            st = sb("st5", [128, 4 * NB5], F32, p5)
            s.dma("sp", lambda e: e.dma_start(out=gt[:], in_=gfin_d[0:1, :].to_broadcast([128, D])), w=["gt"])

            def load5(i):
                b = i % NB5
                s.dma("sp", lambda e: e.dma_start(out=xt[b][:], in_=x1_d[i * 128:(i + 1) * 128, :]), w=["xt%d" % b])

            for i in range(K5):
                load5(i)
            for i in range(NT):
                b = i % NB5
                if i + K5 < NT:
                    load5(i + K5)
                sq, ms, rs = (st[:, 4 * b + k:4 * b + k + 1] for k in range(3))
                tag = "p5_%d" % b
                s.op("act", lambda e: e.activation(out=junk[:], in_=xt[b][:], func=AF.Square, accum_out=sq),
                     r=["xt%d" % b], w=["junk", tag + "ssq"])
                rstd_from_ssq(sq, ms, rs, D, tag)
                s.op("dve", lambda e: e.scalar_tensor_tensor(out=ot[b][:], in0=xt[b][:], scalar=rs, in1=gt[:],
                                                             op0=ALU.mult, op1=ALU.mult),
                     r=["xt%d" % b, tag + "rstd", "gt"], w=["ot%d" % b])
                s.dma("sp", lambda e: e.dma_start(out=out_d[i * 128:(i + 1) * 128, :], in_=ot[b][:]), r=["ot%d" % b], w=["out%d" % i])
        s.barrier()
    return nc


_NC = None


def kernel(x, norm_mix_g, w_in, b_gate, gmlp_norm_g, w_spatial, b_spatial, w_proj_a, w_proj_b, w_out,
           norm_ffn_g, w_router, w_e_gate, w_e_up, w_e_down, norm_final_g):
    global _NC
    f = lambda a: np.ascontiguousarray(np.asarray(a, dtype=np.float32))
    shared = {
        "norm_mix_g": f(norm_mix_g).reshape(1, D),
        "w_in": f(w_in).reshape(D, INC),
        "b_gate": f(b_gate).reshape(16, 128),
        "gmlp_norm_g": f(gmlp_norm_g).reshape(1, 512),
        "w_spatial": f(w_spatial).reshape(8, 128, 128),
        "b_spatial": f(b_spatial).reshape(8, 128),
        "w_proj_a": f(w_proj_a).reshape(512, D),
        "w_proj_b": f(w_proj_b).reshape(512, D),
        "w_out": f(w_out).reshape(D, D),
        "norm_ffn_g": f(norm_ffn_g).reshape(1, D),
        "w_router": f(w_router).reshape(D, NE),
        "w_e_gate": f(w_e_gate).reshape(NE, D, D),
        "w_e_up": f(w_e_up).reshape(NE, D, D),
        "w_e_down": f(w_e_down).reshape(NE, D, D),
        "norm_final_g": f(norm_final_g).reshape(1, D),
    }
    x = f(x)
    if _NC is None:
        _NC = build()
    in_maps = []
    for c in range(8):
        m = dict(shared)
        m["x"] = x[c % 4]
        in_maps.append(m)
    res = run_bass_kernel_spmd(_NC, in_maps, core_ids=list(range(8)))
    if DEBUG:
        return res
    out = np.stack([np.asarray(res.results[c]["out"], dtype=np.float32).reshape(SEQ, D) for c in range(4)], axis=0)
    return out
```

```python
import contextlib
import numpy as np
import concourse.bass as bass
import concourse.mybir as mybir
from concourse.bass_utils import run_bass_kernel_spmd

F32 = mybir.dt.float32
BF16 = mybir.dt.bfloat16
I32 = mybir.dt.int32
AF = mybir.ActivationFunctionType
ALU = mybir.AluOpType
AX = mybir.AxisListType

SEQ = 4096
D = 1024
NT = SEQ // 128
INC = 4608
NE = 16
CAP = 512
ROW = 1024
EPS = 1e-6
PATTERNS = (1, 4, 16)
NPOOL = 12
SKEW = 2
DEBUG = None


class S:
    def __init__(self, nc, es):
        self.nc = nc
        self.e = {"pe": nc.tensor, "act": nc.scalar, "dve": nc.vector, "pool": nc.gpsimd, "sp": nc.sync}
        self.sem = {}
        for k in ("pe", "act", "dve", "pool"):
            self.sem[k] = es.enter_context(nc.semaphore("s_" + k))
        self.cnt = {k: 0 for k in ("pe", "act", "dve", "pool")}
        self.dsem = {}
        self.duse = {}
        self.drr = {"sp": 0, "pool": 0, "act": 0}
        for q in ("sp", "pool", "act"):
            for i in range(NPOOL):
                self.dsem[(q, i)] = es.enter_context(nc.semaphore("d_%s%d" % (q, i)))
                self.duse[(q, i)] = 0
        self.known = {k: {} for k in self.e}
        self.lw = {}
        self.rd = {}

    def _semh(self, key):
        return self.sem[key] if key in self.sem else self.dsem[key]

    def _wait(self, eng, deps):
        need = {}
        for (k, v) in deps:
            if k == eng:
                if eng == "pe":
                    continue
            if v > need.get(k, 0):
                need[k] = v
        kn = self.known[eng]
        for k, v in need.items():
            if kn.get(k, 0) >= v:
                continue
            self.e[eng].wait_ge(self._semh(k), v)
            kn[k] = v

    def _deps(self, r, w):
        deps = []
        for k in r:
            if k in self.lw:
                deps.append(self.lw[k])
        for k in w:
            if k in self.lw:
                deps.append(self.lw[k])
            deps.extend(self.rd.get(k, {}).items())
        return deps

    def _commit(self, ev, r, w):
        for k in r:
            d = self.rd.setdefault(k, {})
            if ev[1] > d.get(ev[0], 0):
                d[ev[0]] = ev[1]
        for k in w:
            self.lw[k] = ev
            self.rd[k] = {}

    def op(self, eng, fn, r=(), w=()):
        self._wait(eng, self._deps(r, w))
        ins = fn(self.e[eng])
        self.cnt[eng] += 1
        ins.then_inc(self.sem[eng], 1)
        ev = (eng, self.cnt[eng])
        self._commit(ev, r, w)
        return ev

    def mm(self, out, lhsT, rhs, start=True, stop=True, r=(), w=(), skip=False):
        return self.op("pe", lambda e: e.matmul(out, lhsT=lhsT, rhs=rhs, start=start, stop=stop, skip_group_check=skip), r, w)

    def tr(self, out, in_, ident, r=(), w=()):
        return self.op("pe", lambda e: e.transpose(out, in_, ident), r, w)

    def dma(self, q, fn, r=(), w=()):
        i = self.drr[q]
        self.drr[q] = (i + 1) % NPOOL
        key = (q, i)
        deps = self._deps(r, w)
        if self.duse[key] > 0:
            deps.append((key, 16 * self.duse[key]))
        self._wait(q, deps)
        ins = fn(self.e[q])
        self.duse[key] += 1
        ins.then_inc(self.dsem[key], 16)
        ev = (key, 16 * self.duse[key])
        self._commit(ev, r, w)
        return ev

    def barrier(self):
        for eng in ("pe", "act", "dve", "pool", "sp"):
            deps = [(k, self.cnt[k]) for k in self.cnt if k != eng and self.cnt[k] > 0]
            deps += [(k, 16 * u) for k, u in self.duse.items() if u > 0]
            kn = self.known[eng]
            for k, v in deps:
                if kn.get(k, 0) >= v:
                    continue
                self.e[eng].wait_ge(self._semh(k), v)
                kn[k] = v
        self.lw = {}
        self.rd = {}


def tokv(ap, r, d, m0, n):
    if d == 1:
        return ap[:, m0:m0 + n]
    return ap[:, r + d * m0: r + d * (m0 + n - 1) + 1: d]


class _Stop(Exception):
    pass


def build():
    try:
        return _build()
    except _Stop as e:
        return e.args[0]


def _build():
    nc = bass.Bass("TRN2", target_bir_lowering=False)
    dt = nc.dram_tensor
    x_d = dt("x", [SEQ, D], F32, kind="ExternalInput").ap()
    gmix_d = dt("norm_mix_g", [1, D], F32, kind="ExternalInput").ap()
    win_d = dt("w_in", [D, INC], F32, kind="ExternalInput").ap()
    bgate_d = dt("b_gate", [16, 128], F32, kind="ExternalInput").ap()
    ggm_d = dt("gmlp_norm_g", [1, 512], F32, kind="ExternalInput").ap()
    wsp_d = dt("w_spatial", [8, 128, 128], F32, kind="ExternalInput").ap()
    bsp_d = dt("b_spatial", [8, 128], F32, kind="ExternalInput").ap()
    wpa_d = dt("w_proj_a", [512, D], F32, kind="ExternalInput").ap()
    wpb_d = dt("w_proj_b", [512, D], F32, kind="ExternalInput").ap()
    wout_d = dt("w_out", [D, D], F32, kind="ExternalInput").ap()
    gffn_d = dt("norm_ffn_g", [1, D], F32, kind="ExternalInput").ap()
    wr_d = dt("w_router", [D, NE], F32, kind="ExternalInput").ap()
    weg_d = dt("w_e_gate", [NE, D, D], F32, kind="ExternalInput").ap()
    weu_d = dt("w_e_up", [NE, D, D], F32, kind="ExternalInput").ap()
    wed_d = dt("w_e_down", [NE, D, D], F32, kind="ExternalInput").ap()
    gfin_d = dt("norm_final_g", [1, D], F32, kind="ExternalInput").ap()
    out_d = dt("out", [SEQ, D], F32, kind="ExternalOutput").ap()
    hT_d = dt("hT_scr", [128, 8, SEQ], BF16, kind="Internal").ap()
    x1_d = dt("x1_scr", [SEQ, D], F32, kind="Internal").ap()
    h2_d = dt("h2_scr", [SEQ, ROW], BF16, kind="Internal").ap()
    aff_d = dt("aff_scr", [SEQ, NE], F32, kind="Internal").ap()
    dbg_d = None
    if DEBUG:
        dbg_d = dt("dbg", [128, 4 * SEQ], F32, kind="ExternalOutput").ap()

    with contextlib.ExitStack() as es:
        s = S(nc, es)

        def ck(n):
            if DEBUG == "p1:%d" % n:
                s.barrier()
                raise _Stop(nc)

        def sb(name, shape, dtype, stack=es):
            return stack.enter_context(nc.sbuf_tensor(name, shape, dtype))

        PSW = [es.enter_context(nc.psum_tensor("psw%d" % i, [128, 1024], F32)) for i in range(4)]
        PS = [PSW[i // 2][:, (i % 2) * 512:(i % 2 + 1) * 512] for i in range(8)]

        def psb(i):
            return PS[i][:, :].bitcast(BF16)

        identb = sb("identb", [128, 128], BF16)
        identf = sb("identf", [128, 128], F32)
        onesb = sb("onesb", [128, 128], BF16)
        iot = sb("iot", [128, 128], F32)
        nhalf = sb("nhalf", [128, 1], F32)
        idxi = sb("idxi", [128, NE, 4], I32)
        st_aff = contextlib.ExitStack()
        affT = sb("affT", [16, SEQ], F32, st_aff)
        st_oT = contextlib.ExitStack()
        oT = sb("oT", [128, 4, SEQ], BF16, st_oT)

        s.op("pool", lambda e: e.iota(iot[:], pattern=[[1, 128]], base=0, channel_multiplier=-1,
                                      allow_small_or_imprecise_dtypes=True), w=["iot"])
        s.op("dve", lambda e: e.tensor_scalar(out=identf[:], in0=iot[:], scalar1=0.0, scalar2=None, op0=ALU.is_equal),
             r=["iot"], w=["identf"])
        s.op("dve", lambda e: e.tensor_copy(out=identb[:], in_=identf[:]), r=["identf"], w=["identb"])
        s.op("pool", lambda e: e.memset(onesb[:], 1.0), w=["onesb"])
        s.op("pool", lambda e: e.memset(nhalf[:], -0.5), w=["nhalf"])

        def rstd_from_ssq(ssq, ms, rstd, n, tag):
            s.op("dve", lambda e: e.tensor_scalar(out=ms, in0=ssq, scalar1=1.0 / n, scalar2=EPS, op0=ALU.mult, op1=ALU.add),
                 r=[tag + "ssq"], w=[tag + "ms"])
            s.op("pool", lambda e: e.tensor_tensor(out=rstd, in0=ms, in1=nhalf[:, 0:1], op=ALU.pow),
                 r=[tag + "ms", "nhalf"], w=[tag + "rstd"])

        with contextlib.ExitStack() as p0:
            NB0, K0 = 4, 3
            gt = sb("gt0", [128, D], F32, p0)
            xt = [sb("xt0_%d" % i, [128, D], F32, p0) for i in range(NB0)]
            xb = [sb("xb0_%d" % i, [128, D], BF16, p0) for i in range(NB0)]
            junk = sb("junk0", [128, D], BF16, p0)
            st = sb("st0", [128, 4 * NB0], F32, p0)
            hTg = [sb("hTg0_%d" % i, [128, 8, 512], BF16, p0) for i in range(2)]
            s.dma("sp", lambda e: e.dma_start(out=gt[:], in_=gmix_d[0:1, :].to_broadcast([128, D])), w=["gt"])

            def load0(i):
                b = i % NB0
                s.dma("sp", lambda e: e.dma_start(out=xt[b][:], in_=x_d[i * 128:(i + 1) * 128, :]), w=["xt%d" % b])

            def stA0(i):
                b = i % NB0
                if i + K0 < NT:
                    load0(i + K0)
                sq, ms, rs = (st[:, 4 * b + k:4 * b + k + 1] for k in range(3))
                tag = "p0_%d" % b
                s.op("act", lambda e: e.activation(out=junk[:], in_=xt[b][:], func=AF.Square, accum_out=sq),
                     r=["xt%d" % b], w=["junk", tag + "ssq"])
                rstd_from_ssq(sq, ms, rs, D, tag)
                s.op("dve", lambda e: e.scalar_tensor_tensor(out=xb[b][:], in0=xt[b][:], scalar=rs, in1=gt[:],
                                                             op0=ALU.mult, op1=ALU.mult),
                     r=["xt%d" % b, tag + "rstd", "gt"], w=["xb%d" % b])

            def stB0(i):
                b = i % NB0
                g = i // 4
                pv = psb(i % 2).rearrange("p (a b) -> p a b", a=8)
                for kc in range(8):
                    s.tr(pv[:, kc, :], xb[b][:, kc * 128:(kc + 1) * 128], identb[:], r=["xb%d" % b, "identb"], w=["ps%d" % (i % 2)])
                s.op("act", lambda e: e.copy(out=hTg[g % 2][:, :, (i % 4) * 128:(i % 4 + 1) * 128], in_=pv),
                     r=["ps%d" % (i % 2)], w=["hTg%d" % (g % 2)])
                if i % 4 == 3:
                    s.dma("sp", lambda e: e.dma_start(out=hT_d[:, :, g * 512:(g + 1) * 512], in_=hTg[g % 2][:]),
                          r=["hTg%d" % (g % 2)], w=["hT_d%d" % g])

            for i in range(K0):
                load0(i)
            stA0(0)
            stA0(1)
            for i in range(NT):
                stB0(i)
                if i + 2 < NT:
                    stA0(i + 2)
        s.barrier()
        if DEBUG == "p0":
            return nc

        pw = contextlib.ExitStack()
        wuv = sb("wuv", [128, 8, 1024], BF16, pw)
        wpa = sb("wpa", [128, 4, 1024], BF16, pw)
        wpb = sb("wpb", [128, 4, 1024], BF16, pw)
        wo = sb("wo", [128, 8, 1024], BF16, pw)
        with contextlib.ExitStack() as p1:
            wq = sb("wq", [128, 3, 8, 128], BF16, p1)
            hg = [sb("hg1_%d" % i, [128, 8, 512], BF16, p1) for i in range(2)]
            qkv = sb("qkv", [128, 3, SEQ], BF16, p1)
            qT, kT, vT = qkv[:, 0, :], qkv[:, 1, :], qkv[:, 2, :]
            Vl = sb("Vl", [128, 32, 128], BF16, p1)
            PT = [sb("PT%d" % i, [128, 2, 256], BF16, p1) for i in range(4)]
            EX = [sb("EX%d" % i, [128, 2, 256], BF16, p1) for i in range(3)]
            msk = sb("msk", [128, 12, 2, 256], BF16, p1)
            accN = sb("accN", [128, SEQ], F32, p1)
            accD = sb("accD", [128, SEQ], F32, p1)
            scr1 = sb("scr1", [128, 768], F32, p1)
            dlt, band, mtmp = scr1[:, 0:256], scr1[:, 256:512], scr1[:, 512:768]
            dtmpB = sb("dtmpB", [128, 512], F32, p1)
            DT = [scr1[:, 0:512].rearrange("p (a b) -> p a b", a=2), dtmpB[:, :].rearrange("p (a b) -> p a b", a=2)]
            DTK = [["dt0", "dlt", "band"], ["dt1"]]

            s.op("pool", lambda e: e.iota(dlt, pattern=[[-1, 256]], base=64, channel_multiplier=1,
                                          allow_small_or_imprecise_dtypes=True), w=["dlt"])
            s.op("dve", lambda e: e.tensor_scalar(out=band, in0=dlt, scalar1=-1.0, scalar2=None, op0=ALU.mult), r=["dlt"], w=["band"])
            s.op("dve", lambda e: e.tensor_tensor(out=dlt, in0=dlt, in1=band, op=ALU.max), r=["dlt", "band"], w=["dlt"])
            s.op("dve", lambda e: e.tensor_scalar(out=band, in0=dlt, scalar1=64.5, scalar2=None, op0=ALU.is_le),
                 r=["dlt"], w=["band"])
            for pi, dil in enumerate(PATTERNS):
                for hp in range(4):
                    for hh in range(2):
                        h = hp * 2 + hh
                        slope = 2.0 ** (-8.0 * (h + 1) / 8.0)
                        s.op("act", lambda e: e.activation(out=mtmp, in_=dlt, func=AF.Exp, scale=-slope * dil),
                             r=["dlt"], w=["mtmp"])
                        s.op("dve", lambda e: e.tensor_tensor(out=msk[:, pi * 4 + hp, hh, :], in0=mtmp, in1=band, op=ALU.mult),
                             r=["mtmp", "band"], w=["msk"])

            ck(1)
            for hp in range(4):
                for t3 in range(3):
                    c0 = 1024 + 512 * t3 + hp * 128
                    s.dma("pool", lambda e: e.dma_start(out=wq[:, t3, :, :],
                                                        in_=win_d[:, c0:c0 + 128].rearrange("(kc p) c -> p kc c", p=128)),
                          w=["wq"])
                if hp == 0:
                    castw = lambda dst, src, key: s.dma("pool", lambda e: e.dma_start(out=dst, in_=src), w=[key])
                    castw(wuv[:], win_d[:, 0:1024].rearrange("(kc p) c -> p kc c", p=128), "wuv")
                    castw(wpa[:], wpa_d.rearrange("(kc p) c -> p kc c", p=128), "wpa")
                    castw(wpb[:], wpb_d.rearrange("(kc p) c -> p kc c", p=128), "wpb")
                    castw(wo[:], wout_d.rearrange("(kc p) c -> p kc c", p=128), "wo")
                for g in range(8):
                    hb = g % 2
                    s.dma("sp", lambda e: e.dma_start(out=hg[hb][:], in_=hT_d[:, :, g * 512:(g + 1) * 512]),
                          r=["hT_d"], w=["hg%d" % hb])
                    for t3 in range(3):
                        bank = (g * 3 + t3) % 2
                        for kc in range(8):
                            s.mm(PS[bank][:, :], lhsT=wq[:, t3, kc, :], rhs=hg[hb][:, kc, :], start=(kc == 0), stop=(kc == 7),
                                 r=["wq", "hg%d" % hb], w=["ps%d" % bank])
                        s.op("act", lambda e: e.copy(out=qkv[:, t3, g * 512:(g + 1) * 512], in_=PS[bank][:, :]),
                             r=["ps%d" % bank], w=["qkv%d" % t3])
                ck(2)
                s.op("pool", lambda e: e.memset(accN[:], 0.0), w=["accN"])
                s.op("pool", lambda e: e.memset(accD[:], 0.0), w=["accD"])
                blk_ctr = 0
                grp_ctr = 0
                pend = []

                def flushB(keep):
                    while len(pend) > keep:
                        pend.pop(0)()

                def mkA(pi, dil, r, j, L, bc):
                    def fn():
                        qa = max(0, 128 * j - 64)
                        qb_ = min(L, 128 * j + 192)
                        c0 = qa - (128 * j - 64)
                        n = qb_ - qa
                        sw = bc % 2
                        spv = PSW[sw][:, :].rearrange("p (a b) -> p a b", a=2)
                        skeys = ["ps%d" % (2 * sw), "ps%d" % (2 * sw + 1)]
                        for hh in range(2):
                            lo, hi = hh * 64, hh * 64 + 64
                            s.mm(spv[:, hh, c0:c0 + n], lhsT=tokv(kT[lo:hi], r, dil, j * 128, 128),
                                 rhs=tokv(qT[lo:hi], r, dil, qa, n), r=["qkv0", "qkv1"], w=[skeys[hh]])
                        ex = EX[bc % 3]
                        pt = PT[bc % 4]
                        s.op("act", lambda e: e.activation(out=ex[:, :, c0:c0 + n], in_=spv[:, :, c0:c0 + n], func=AF.Exp, scale=0.125),
                             r=skeys, w=["EX%d" % (bc % 3)])
                        s.op("dve", lambda e: e.tensor_tensor(out=pt[:, :, c0:c0 + n], in0=ex[:, :, c0:c0 + n],
                                                               in1=msk[:, pi * 4 + hp, :, c0:c0 + n], op=ALU.mult),
                             r=["EX%d" % (bc % 3), "msk"], w=["PT%d" % (bc % 4)])
                    return fn

                def mkB(pi, dil, r, i, L, nb, sl, gcn):
                    def fn():
                        ma = max(0, 128 * i - 64)
                        mb = min(L, 128 * i + 64)
                        n = mb - ma
                        js = [jj for jj in (i - 1, i) if 0 <= jj < nb]
                        nbank = 4 + gcn % 2
                        dbank = 6 + gcn % 2
                        nv = PS[nbank][:, :].rearrange("p (a b) -> p a b", a=2)
                        dv = PS[dbank][:, :].rearrange("p (a b) -> p a b", a=2)
                        col = (i % 2) * 128 + (ma - (128 * i - 64))
                        for ji, jj in enumerate(js):
                            c = ma - (128 * jj - 64)
                            ptj = PT[sl[jj]]
                            s.mm(nv[:, :, col:col + n], lhsT=Vl[:, r * nb + jj, :], rhs=ptj[:, :, c:c + n],
                                 start=(ji == 0), stop=(ji == len(js) - 1),
                                 r=["Vl", "PT%d" % sl[jj]], w=["ps%d" % nbank])
                        for ji, jj in enumerate(js):
                            c = ma - (128 * jj - 64)
                            ptj = PT[sl[jj]]
                            s.mm(dv[:, :, col:col + n], lhsT=onesb[:], rhs=ptj[:, :, c:c + n],
                                 start=(ji == 0), stop=(ji == len(js) - 1),
                                 r=["onesb", "PT%d" % sl[jj]], w=["ps%d" % dbank])
                        if i % 2 == 1 or i == nb:
                            g = i // 2
                            ga = max(0, 256 * g - 64)
                            gb_ = min(L, 256 * g + 192)
                            gn = gb_ - ga
                            gc = ga - (256 * g - 64)
                            dtv = DT[gcn % 2]
                            dtk = DTK[gcn % 2]
                            s.op("act", lambda e: e.copy(out=dtv[:, :, gc:gc + gn], in_=dv[:, :, gc:gc + gn]),
                                 r=["ps%d" % dbank], w=dtk)
                            for hh in range(2):
                                lo, hi = hh * 64, hh * 64 + 64
                                av = tokv(accN[lo:hi], r, dil, ga, gn)
                                s.op("dve", lambda e: e.tensor_tensor(out=av, in0=av, in1=nv[lo:hi, hh, gc:gc + gn], op=ALU.add),
                                     r=["ps%d" % nbank, "accN"], w=["accN"])
                                ad = tokv(accD[lo:hi], r, dil, ga, gn)
                                s.op("pool", lambda e: e.tensor_tensor(out=ad, in0=ad, in1=dtv[lo:hi, hh, gc:gc + gn], op=ALU.add),
                                     r=[dtk[0], "accD"], w=["accD"])
                    return fn

                for pi, dil in enumerate(PATTERNS):
                    L = SEQ // dil
                    nb = L // 128
                    flushB(0)
                    for r in range(dil):
                        for j in range(nb):
                            blk = r * nb + j
                            pvw = psb(2).rearrange("p (a b) -> p a b", a=8)
                            s.tr(pvw[:, blk % 8, :], tokv(vT, r, dil, j * 128, 128), identb[:], r=["qkv2", "identb"], w=["ps2"])
                            if blk % 8 == 7:
                                s.op("act", lambda e: e.copy(out=Vl[:, blk - 7:blk + 1, :], in_=pvw), r=["ps2"], w=["Vl"])
                    for r in range(dil):
                        slots = {}
                        for j in range(nb + 1):
                            if j < nb:
                                mkA(pi, dil, r, j, L, blk_ctr)()
                                slots[j] = blk_ctr % 4
                                blk_ctr += 1
                            pend.append(mkB(pi, dil, r, j, L, nb, dict(slots), grp_ctr))
                            if j % 2 == 1 or j == nb:
                                grp_ctr += 1
                            flushB(SKEW)
                flushB(0)
                for hf in range(2):
                    sl = slice(hf * 2048, (hf + 1) * 2048)
                    s.op("dve", lambda e: e.reciprocal(out=accD[:, sl], in_=accD[:, sl]), r=["accD"], w=["accD"])
                    s.op("dve", lambda e: e.tensor_tensor(out=oT[:, hp, sl], in0=accN[:, sl], in1=accD[:, sl], op=ALU.mult),
                         r=["accN", "accD"], w=["oT"])
        s.barrier()
        if DEBUG == "oT":
            with contextlib.ExitStack() as pd:
                tmpf = sb("dbgtmp", [128, SEQ], F32, pd)
                for hp in range(4):
                    s.op("dve", lambda e: e.tensor_copy(out=tmpf[:], in_=oT[:, hp, :]), r=["oT"], w=["tmpf"])
                    s.dma("sp", lambda e: e.dma_start(out=dbg_d[:, hp * SEQ:(hp + 1) * SEQ], in_=tmpf[:]), r=["tmpf"], w=["dbg"])
                s.barrier()
            return nc

        with contextlib.ExitStack() as p2:
            wgg = sb("wgg", [128, 8, 2048], BF16, p2)
            wrt = sb("wrt", [128, 8, NE], BF16, p2)
            wsf = sb("wsf", [128, 8, 128], F32, p2)
            wsT = sb("wsT", [128, 8, 128], BF16, p2)
            bspr = sb("bspr", [8, 128], F32, p2)
            bspt = sb("bspt", [128, 8], F32, p2)
            bg = sb("bg", [128, 16], F32, p2)
            bgr = sb("bgr", [16, 128], F32, p2)
            ggt = sb("ggt", [128, 512], F32, p2)
            g2t = sb("g2t", [128, D], F32, p2)
            hg = [sb("hg2_%d" % i, [128, 8, 512], BF16, p2) for i in range(2)]
            gu = sb("gu", [128, 512], BF16, p2)
            gv = sb("gv", [128, 512], F32, p2)
            vn = sb("vn", [128, 512], BF16, p2)
            ab = sb("ab", [128, 512], BF16, p2)
            aT = [sb("aT%d" % i, [128, 4, 512], BF16, p2) for i in range(2)]
            sga = sb("sga", [128, 512], F32, p2)
            sgb = sb("sgb", [128, 512], F32, p2)
            t1 = sga
            t2 = sgb
            mT = [sb("mT%d" % i, [128, 8, 512], BF16, p2) for i in range(2)]
            x1 = [sb("x1_%d" % i, [128, D], F32, p2) for i in range(2)]
            hrow = [sb("hrow%d" % i, [128, ROW], BF16, p2) for i in range(2)]
            h2T = sb("h2T", [128, 8, 128], BF16, p2)
            st = sb("st2", [128, 16], F32, p2)
            lg = sb("lg", [128, NE], F32, p2)
            aff = sb("aff", [128, NE], F32, p2)

            cast = lambda dst, src, key: s.dma("pool", lambda e: e.dma_start(out=dst, in_=src), w=[key])
            s.dma("sp", lambda e: e.dma_start(out=wsf[:], in_=wsp_d.rearrange("g t s -> t g s")), w=["wsf"])
            s.dma("sp", lambda e: e.dma_start(out=bspr[:], in_=bsp_d), w=["bspr"])
            s.dma("sp", lambda e: e.dma_start(out=bgr[:], in_=bgate_d), w=["bgr"])
            s.dma("sp", lambda e: e.dma_start(out=ggt[:], in_=ggm_d[0:1, :].to_broadcast([128, 512])), w=["ggt"])
            s.dma("sp", lambda e: e.dma_start(out=g2t[:], in_=gffn_d[0:1, :].to_broadcast([128, D])), w=["g2t"])
            cast(wgg[:], win_d[:, 2560:4608].rearrange("(kc p) c -> p kc c", p=128), "wgg")
            cast(wrt[:], wr_d.rearrange("(kc p) c -> p kc c", p=128), "wrt")
            for g in range(8):
                s.tr(PS[0][:, 0:128], wsf[:, g, :], identf[:], r=["wsf", "identf"], w=["ps0"])
                s.op("dve", lambda e: e.tensor_copy(out=wsT[:, g, :], in_=PS[0][:, 0:128]), r=["ps0"], w=["wsT"])
            s.tr(PS[0][:, 0:8], bspr[:, :], identf[0:8, 0:8], r=["bspr", "identf"], w=["ps0"])
            s.op("dve", lambda e: e.tensor_copy(out=bspt[:], in_=PS[0][:, 0:8]), r=["ps0"], w=["bspt"])
            s.tr(PS[0][:, 0:16], bgr[:, :], identf[0:16, 0:16], r=["bgr", "identf"], w=["ps0"])
            s.op("dve", lambda e: e.tensor_copy(out=bg[:], in_=PS[0][:, 0:16]), r=["ps0"], w=["bg"])

            def Ia(g, t):
                hb = g % 2
                tsl = slice(t * 128, (t + 1) * 128)
                if t == 0:
                    s.dma("act", lambda e: e.dma_start(out=hg[hb][:], in_=hT_d[:, :, g * 512:(g + 1) * 512]), r=["hT_d"], w=["hg%d" % hb])
                for half in range(2):
                    for kc in range(8):
                        s.mm(PS[half][:, :], lhsT=hg[hb][:, kc, tsl], rhs=wuv[:, kc, half * 512:(half + 1) * 512],
                             start=(kc == 0), stop=(kc == 7), r=["hg%d" % hb, "wuv"], w=["ps%d" % half])
                s.op("act", lambda e: e.activation(out=gu[:], in_=PS[0][:, :], func=AF.Gelu_apprx_tanh), r=["ps0"], w=["gu"])
                s.op("act", lambda e: e.activation(out=gv[:], in_=PS[1][:, :], func=AF.Gelu_apprx_tanh), r=["ps1"], w=["gv"])
                s.op("act", lambda e: e.activation(out=vn[:], in_=gv[:], func=AF.Square, accum_out=st[:, 0:1]),
                     r=["gv"], w=["vn", "gmssq"])
                rstd_from_ssq(st[:, 0:1], st[:, 1:2], st[:, 2:3], 512, "gm")
                s.op("dve", lambda e: e.scalar_tensor_tensor(out=vn[:], in0=gv[:], scalar=st[:, 2:3], in1=ggt[:],
                                                             op0=ALU.mult, op1=ALU.mult),
                     r=["gv", "gmrstd", "ggt"], w=["vn"])

            def Ib(g, t):
                for gg in range(8):
                    s.mm(PS[2][:, gg * 64:(gg + 1) * 64], lhsT=wsT[:, gg, :], rhs=vn[:, gg * 64:(gg + 1) * 64],
                         r=["wsT", "vn"], w=["ps2"])
                s.op("dve", lambda e: e.tensor_tensor(out=gv[:].rearrange("p (a b) -> p a b", a=8),
                                                      in0=PS[2][:, :].rearrange("p (a b) -> p a b", a=8),
                                                      in1=bspt[:].unsqueeze(2).to_broadcast([128, 8, 64]), op=ALU.add),
                     r=["ps2", "bspt"], w=["gv"])
                s.op("dve", lambda e: e.tensor_tensor(out=ab[:], in0=gv[:], in1=gu[:], op=ALU.mult), r=["gv", "gu"], w=["ab"])

            def Ic(g, t):
                tsl = slice(t * 128, (t + 1) * 128)
                pv = psb(3).rearrange("p (a b) -> p a b", a=8)
                for kc in range(4):
                    s.tr(pv[:, kc, :], ab[:, kc * 128:(kc + 1) * 128], identb[:], r=["ab", "identb"], w=["ps3"])
                s.op("act", lambda e: e.copy(out=aT[g % 2][:, :, tsl], in_=pv[:, 0:4, :]), r=["ps3"], w=["aT%d" % (g % 2)])

            def II(g, c):
                hb = g % 2
                gsl = slice(g * 512, (g + 1) * 512)
                csl = slice(c * 128, (c + 1) * 128)
                aTg = aT[g % 2]
                for kc in range(8):
                    s.mm(PS[4][:, :], lhsT=wgg[:, kc, csl], rhs=hg[hb][:, kc, :], start=(kc == 0), stop=(kc == 7),
                         r=["wgg", "hg%d" % hb], w=["ps4"])
                for kc in range(8):
                    s.mm(PS[5][:, :], lhsT=wgg[:, kc, 1024 + c * 128:1024 + (c + 1) * 128], rhs=hg[hb][:, kc, :],
                         start=(kc == 0), stop=(kc == 7), r=["wgg", "hg%d" % hb], w=["ps5"])
                for kc in range(4):
                    s.mm(PS[6][:, :], lhsT=wpa[:, kc, csl], rhs=aTg[:, kc, :], start=(kc == 0), stop=(kc == 3),
                         r=["wpa", "aT%d" % (g % 2)], w=["ps6"])
                for kc in range(4):
                    s.mm(PS[7][:, :], lhsT=wpb[:, kc, csl], rhs=oT[:, kc, gsl], start=(kc == 0), stop=(kc == 3),
                         r=["wpb", "oT"], w=["ps7"])
                s.op("act", lambda e: e.activation(out=sga[:], in_=PS[4][:, :], func=AF.Sigmoid, bias=bg[:, c:c + 1], scale=1.0),
                     r=["ps4", "bg"], w=["sga"])
                s.op("act", lambda e: e.activation(out=sgb[:], in_=PS[5][:, :], func=AF.Sigmoid, bias=bg[:, 8 + c:9 + c], scale=1.0),
                     r=["ps5", "bg"], w=["sgb"])
                s.op("dve", lambda e: e.tensor_tensor(out=t1[:], in0=PS[6][:, :], in1=sga[:], op=ALU.mult), r=["ps6", "sga"], w=["sga"])
                s.op("dve", lambda e: e.tensor_tensor(out=t2[:], in0=PS[7][:, :], in1=sgb[:], op=ALU.mult), r=["ps7", "sgb"], w=["sgb"])
                s.op("pool", lambda e: e.tensor_tensor(out=mT[g % 2][:, c, :], in0=t1[:], in1=t2[:], op=ALU.add),
                     r=["sga", "sgb"], w=["mT%d" % (g % 2)])

            def IIIa(g, t):
                i = g * 4 + t
                b = i % 2
                tsl = slice(t * 128, (t + 1) * 128)
                s.dma("act", lambda e: e.dma_start(out=x1[b][:], in_=x_d[i * 128:(i + 1) * 128, :]), w=["x1_%d" % b])
                for half in range(2):
                    for kc in range(8):
                        s.mm(PS[half][:, :], lhsT=mT[g % 2][:, kc, tsl], rhs=wo[:, kc, half * 512:(half + 1) * 512],
                             start=(kc == 0), stop=(kc == 7), r=["mT%d" % (g % 2), "wo"], w=["ps%d" % half])
                    hs = slice(half * 512, (half + 1) * 512)
                    s.op("dve", lambda e: e.tensor_tensor(out=x1[b][:, hs], in0=PS[half][:, :], in1=x1[b][:, hs], op=ALU.add),
                         r=["ps%d" % half, "x1_%d" % b], w=["x1_%d" % b])
                s.dma("sp", lambda e: e.dma_start(out=x1_d[i * 128:(i + 1) * 128, :], in_=x1[b][:]), r=["x1_%d" % b], w=["x1_d%d" % i])
                s.op("act", lambda e: e.activation(out=hrow[b][:, 0:D], in_=x1[b][:], func=AF.Square, accum_out=st[:, 4:5]),
                     r=["x1_%d" % b], w=["hrow%d" % b, "rtssq"])
                rstd_from_ssq(st[:, 4:5], st[:, 5:6], st[:, 6:7], D, "rt")
                s.op("dve", lambda e: e.scalar_tensor_tensor(out=hrow[b][:, 0:D], in0=x1[b][:], scalar=st[:, 6:7], in1=g2t[:],
                                                             op0=ALU.mult, op1=ALU.mult),
                     r=["x1_%d" % b, "rtrstd", "g2t"], w=["hrow%d" % b])

            def IIIb1(g, t):
                i = g * 4 + t
                b = i % 2
                pv = psb(3).rearrange("p (a b) -> p a b", a=8)
                for kc in range(8):
                    s.tr(pv[:, kc, :], hrow[b][:, kc * 128:(kc + 1) * 128], identb[:], r=["hrow%d" % b, "identb"], w=["ps3"])
                s.op("act", lambda e: e.copy(out=h2T[:], in_=pv), r=["ps3"], w=["h2T"])
                s.dma("sp", lambda e: e.dma_start(out=h2_d[i * 128:(i + 1) * 128, :], in_=hrow[b][:]), r=["hrow%d" % b], w=["h2_d%d" % i])

            def IIIb2(g, t):
                i = g * 4 + t
                for kc in range(8):
                    s.mm(PS[2][:, 0:NE], lhsT=h2T[:, kc, :], rhs=wrt[:, kc, :], start=(kc == 0), stop=(kc == 7),
                         r=["h2T", "wrt"], w=["ps2"])
                s.op("dve", lambda e: e.tensor_reduce(out=st[:, 8:9], in_=PS[2][:, 0:NE], axis=AX.X, op=ALU.max), r=["ps2"], w=["smx"])
                s.op("dve", lambda e: e.tensor_scalar(out=st[:, 9:10], in0=st[:, 8:9], scalar1=-1.0, scalar2=None, op0=ALU.mult),
                     r=["smx"], w=["snmx"])
                s.op("act", lambda e: e.activation(out=lg[:], in_=PS[2][:, 0:NE], func=AF.Exp, bias=st[:, 9:10], scale=1.0,
                                                   accum_out=st[:, 10:11]),
                     r=["ps2", "snmx"], w=["lg", "ssum"])
                s.op("dve", lambda e: e.reciprocal(out=st[:, 11:12], in_=st[:, 10:11]), r=["ssum"], w=["srs"])
                s.op("dve", lambda e: e.tensor_scalar(out=aff[:], in0=lg[:], scalar1=st[:, 11:12], scalar2=None, op0=ALU.mult),
                     r=["lg", "srs"], w=["aff"])
                s.dma("sp", lambda e: e.dma_start(out=aff_d[i * 128:(i + 1) * 128, :], in_=aff[:]), r=["aff"], w=["aff_d%d" % i])

            def IIIc(g, t):
                i = g * 4 + t
                s.tr(PS[2][0:16, 128:256], aff[:, :], identf[:], r=["aff", "identf"], w=["ps2"])
                s.op("act", lambda e: e.copy(out=affT[:, i * 128:(i + 1) * 128], in_=PS[2][0:16, 128:256]), r=["ps2"], w=["affT"])

            NSLOT = 8 * 10 + 12
            pre = [[] for _ in range(NSLOT)]
            mid = [[] for _ in range(NSLOT)]
            post = [[] for _ in range(NSLOT)]
            last = [[] for _ in range(NSLOT)]
            mk = lambda f, g, t: (lambda: f(g, t))
            for g in range(8):
                for t in range(4):
                    b0 = 8 * g
                    last[b0 + 2 * t].append(mk(Ia, g, t))
                    post[b0 + 2 * t + 1].append(mk(Ib, g, t))
                    pre[b0 + 2 * t + 2].append(mk(Ic, g, t))
                    b2 = 8 * (g + 2)
                    last[b2 + 2 * t + 1].append(mk(IIIa, g, t))
                    post[b2 + 2 * t + 2].append(mk(IIIb1, g, t))
                    pre[b2 + 2 * t + 3].append(mk(IIIb2, g, t))
                    post[b2 + 2 * t + 3].append(mk(IIIc, g, t))
                for c in range(8):
                    mid[8 * (g + 1) + c].append(mk(II, g, c))
            for sl in range(NSLOT):
                for lst in (pre[sl], mid[sl], post[sl], last[sl]):
                    for fn in lst:
                        fn()
        pw.close()
        st_oT.close()
        s.barrier()

        with contextlib.ExitStack() as p3:
            cmp_ = sb("cmp", [16, SEQ], F32, p3)
            ones16 = sb("ones16", [16, SEQ], F32, p3)
            cs = sb("cs", [16, SEQ], F32, p3)
            bs = sb("bs", [16, 8], F32, p3)
            csT = sb("csT", [128, NT * NE], F32, p3)
            csI = sb("csI", [128, NT * NE], I32, p3)
            aI = sb("aI", [128, NT * NE], I32, p3)
            bI = sb("bI", [128, NT * NE], I32, p3)
            aF = sb("aF", [128, NT, NE], BF16, p3)
            bF = sb("bF", [128, NT, NE], F32, p3)
            io4 = sb("io4", [128, 4], F32, p3)
            ones4 = sb("ones4", [128, NE, 4], BF16, p3)
            EQ = [sb("EQ%d" % i, [128, NE, 128], BF16, p3) for i in range(2)]
            LT = [sb("LT%d" % i, [128, NE, 128], BF16, p3) for i in range(2)]
            LEb = [sb("LEb%d" % i, [128, NE, 4], BF16, p3) for i in range(2)]
            idxf = sb("idxf", [128, NE * 4], F32, p3)
            lo, hi, mid, cnt, ge, dd = (bs[:, k:k + 1] for k in range(6))
            s.op("dve", lambda e: e.memset(bs[:, 0:1], 0.0), w=["lo"])
            s.op("dve", lambda e: e.memset(bs[:, 1:2], 1.0), w=["hi"])
            s.op("pool", lambda e: e.memset(ones16[:], 1.0), w=["ones16"])
            s.op("pool", lambda e: e.memset(ones4[:], 1.0), w=["ones4"])
            s.op("pool", lambda e: e.iota(io4[:], pattern=[[1, 4]], base=0, channel_multiplier=0,
                                          allow_small_or_imprecise_dtypes=True), w=["io4"])
            for it in range(30):
                wk = 0.5 ** (it + 1)
                s.op("dve", lambda e: e.tensor_scalar(out=mid, in0=lo, scalar1=wk, scalar2=None, op0=ALU.add), r=["lo"], w=["mid"])
                s.op("dve", lambda e: e.tensor_scalar(out=cmp_[:], in0=affT[:], scalar1=mid, scalar2=0.0, op0=ALU.is_ge, op1=ALU.add,
                                                      accum_out=cnt),
                     r=["affT", "mid"], w=["cmp", "cnt"])
                s.op("dve", lambda e: e.tensor_scalar(out=ge, in0=cnt, scalar1=CAP - 0.5, scalar2=None, op0=ALU.is_ge), r=["cnt"], w=["ge"])
                s.op("dve", lambda e: e.scalar_tensor_tensor(out=lo, in0=ge, scalar=wk, in1=lo, op0=ALU.mult, op1=ALU.add),
                     r=["ge", "lo"], w=["lo"])
            s.op("dve", lambda e: e.tensor_scalar(out=cmp_[:], in0=affT[:], scalar1=lo, scalar2=None, op0=ALU.is_ge), r=["affT", "lo"], w=["cmp"])
            s.op("dve", lambda e: e.tensor_tensor_scan(out=cs[:], data0=ones16[:], data1=cmp_[:], initial=0.0, op0=ALU.mult, op1=ALU.add),
                 r=["cmp", "ones16"], w=["cs"])
            for i in range(NT):
                s.tr(PS[0][:, i * NE:(i + 1) * NE], cs[:, i * 128:(i + 1) * 128], identf[0:16, 0:16], r=["cs", "identf"], w=["ps0"])
            s.op("dve", lambda e: e.tensor_copy(out=csT[:], in_=PS[0][:, :]), r=["ps0"], w=["csT"])
            s.op("dve", lambda e: e.tensor_copy(out=csI[:], in_=csT[:]), r=["csT"], w=["csI"])
            s.op("dve", lambda e: e.tensor_single_scalar(out=aI[:], in_=csI[:], scalar=2, op=ALU.arith_shift_right), r=["csI"], w=["aI"])
            s.op("dve", lambda e: e.tensor_single_scalar(out=bI[:], in_=csI[:], scalar=3, op=ALU.bitwise_and), r=["csI"], w=["bI"])
            s.op("dve", lambda e: e.tensor_copy(out=aF[:].rearrange("p a b -> p (a b)"), in_=aI[:]), r=["aI"], w=["aF"])
            s.op("dve", lambda e: e.tensor_copy(out=bF[:].rearrange("p a b -> p (a b)"), in_=bI[:]), r=["bI"], w=["bF"])
            ioc = sb("ioc", [128, 128], F32, p3)
            s.op("pool", lambda e: e.iota(ioc[:], pattern=[[1, 128]], base=0, channel_multiplier=0,
                                          allow_small_or_imprecise_dtypes=True), w=["ioc"])
            iocb = sb("iocb", [128, 128], BF16, p3)
            s.op("dve", lambda e: e.tensor_copy(out=iocb[:], in_=ioc[:]), r=["ioc"], w=["iocb"])
            idv = PS[1][:, 0:NE * 4].rearrange("p (a b) -> p a b", a=NE)
            for i in range(NT):
                b = i % 2
                s.op("dve", lambda e: e.tensor_tensor(out=EQ[b][:], in0=aF[:, i, :].unsqueeze(2).to_broadcast([128, NE, 128]),
                                                      in1=iocb[:].unsqueeze(1).to_broadcast([128, NE, 128]), op=ALU.is_equal),
                     r=["aF", "iocb"], w=["EQ%d" % b])
                s.op("dve", lambda e: e.tensor_tensor(out=LT[b][:], in0=aF[:, i, :].unsqueeze(2).to_broadcast([128, NE, 128]),
                                                       in1=iocb[:].unsqueeze(1).to_broadcast([128, NE, 128]), op=ALU.is_lt),
                     r=["aF", "iocb"], w=["LT%d" % b])
                s.op("dve", lambda e: e.tensor_tensor(out=LEb[b][:], in0=bF[:, i, :].unsqueeze(2).to_broadcast([128, NE, 4]),
                                                      in1=io4[:].unsqueeze(1).to_broadcast([128, NE, 4]), op=ALU.is_le),
                     r=["bF", "io4"], w=["LEb%d" % b])
                for ex in range(NE):
                    s.mm(idv[:, ex, :], lhsT=EQ[b][:, ex, :], rhs=LEb[b][:, ex, :], start=(i == 0 and ex == 0), stop=False,
                         r=["EQ%d" % b, "LEb%d" % b], w=["ps1"], skip=True)
                    s.mm(idv[:, ex, :], lhsT=LT[b][:, ex, :], rhs=ones4[:, ex, :], start=False, stop=(i == NT - 1),
                         r=["LT%d" % b, "ones4"], w=["ps1"], skip=True)
            s.op("dve", lambda e: e.tensor_copy(out=idxf[:], in_=PS[1][:, 0:NE * 4]), r=["ps1"], w=["idxf"])
            s.op("dve", lambda e: e.tensor_copy(out=idxi[:].rearrange("p a b -> p (a b)"), in_=idxf[:]), r=["idxf"], w=["idxi"])
        s.barrier()
        if DEBUG == "idx":
            with contextlib.ExitStack() as pd:
                tmpf = sb("dbgtmp", [128, SEQ], F32, pd)
                s.op("pool", lambda e: e.memset(tmpf[:], 0.0), w=["tmpf"])
                s.op("dve", lambda e: e.tensor_copy(out=tmpf[:, 0:64], in_=idxi[:].rearrange("p a b -> p (a b)")), r=["idxi"], w=["tmpf"])
                s.dma("sp", lambda e: e.dma_start(out=dbg_d[:, 0:SEQ], in_=tmpf[:]), r=["tmpf"], w=["dbg"])
                s.op("dve", lambda e: e.tensor_copy(out=tmpf[0:16, :], in_=affT[:]), r=["affT"], w=["tmpf"])
                s.dma("sp", lambda e: e.dma_start(out=dbg_d[:, SEQ:2 * SEQ], in_=tmpf[:]), r=["tmpf"], w=["dbg"])
                s.barrier()
            return nc

        st_aff.close()
        with contextlib.ExitStack() as p4:
            wg = [sb("wg%d" % i, [128, 8, D], BF16, p4) for i in range(2)]
            wu = [sb("wu%d" % i, [128, 8, D], BF16, p4) for i in range(2)]
            wd = [sb("wd%d" % i, [128, 8, D], BF16, p4) for i in range(2)]
            Xe = [sb("Xe%d" % i, [128, 4, ROW], BF16, p4) for i in range(2)]
            XeT = sb("XeT", [128, 8, CAP], BF16, p4)
            hid = sb("hid", [128, 8, CAP], BF16, p4)
            sg = sb("sg", [128, CAP], F32, p4)
            ye = [sb("ye%d" % i, [128, D], F32, p4) for i in range(2)]
            Ge = [sb("Ge%d" % i, [128, 4, NE], F32, p4) for i in range(2)]

            NSTG = 6
            stg = [sb("stg%d" % i, [128, 2, D], F32, p4) for i in range(NSTG)]
            chunks = [(ex_, mi, q) for ex_ in range(NE) for mi in range(3) for q in range(4)]
            mats = ((wg, weg_d, "wg"), (wu, weu_d, "wu"), (wd, wed_d, "wd"))

            def emit_dma(n):
                ex_, mi, q = chunks[n]
                k = n % NSTG
                src = mats[mi][1]
                s.dma("sp", lambda e: e.dma_start(out=stg[k][:], in_=src[ex_, q * 256:(q + 1) * 256, :].rearrange("(kc p) c -> p kc c", p=128)),
                      w=["stg%d" % k])

            def emit_cast(n):
                ex_, mi, q = chunks[n]
                k = n % NSTG
                b_ = ex_ % 2
                dst = mats[mi][0][b_]
                wkey = "%s%d_%d" % (mats[mi][2], b_, q)
                if n % 2 == 0:
                    s.op("act", lambda e: e.copy(out=dst[:, 2 * q:2 * q + 2, :], in_=stg[k][:]), r=["stg%d" % k], w=[wkey])
                else:
                    s.op("dve", lambda e: e.tensor_copy(out=dst[:, 2 * q:2 * q + 2, :], in_=stg[k][:]), r=["stg%d" % k], w=[wkey])
                if n + NSTG < len(chunks):
                    emit_dma(n + NSTG)

            def load_w(ex_):
                return [(lambda n=n: emit_cast(n)) for n in range(12 * ex_, 12 * ex_ + 12)]

            for n in range(NSTG):
                emit_dma(n)

            def gather(ex):
                b = ex % 2
                for c0 in range(4):
                    s.dma("pool", lambda e: e.indirect_dma_start(out=Xe[b][:, c0, :], out_offset=None, in_=h2_d[:, :],
                                                                 in_offset=bass.IndirectOffsetOnAxis(ap=idxi[:, ex, c0:c0 + 1], axis=0)),
                          r=["idxi", "h2_d"], w=["Xe%d_%d" % (b, c0)])
                    s.dma("pool", lambda e: e.indirect_dma_start(out=Ge[b][:, c0, :], out_offset=None, in_=aff_d[:, :],
                                                                 in_offset=bass.IndirectOffsetOnAxis(ap=idxi[:, ex, c0:c0 + 1], axis=0)),
                          r=["idxi", "aff_d"], w=["Ge%d_%d" % (b, c0)])

            for fn in load_w(0):
                fn()
            gather(0)
            yctr = 0
            for ex in range(NE):
                b = ex % 2
                casts = []
                if ex + 1 < NE:
                    casts = load_w(ex + 1)
                    gather(ex + 1)
                for c0 in range(4):
                    pv = psb(c0 % 2).rearrange("p (a b) -> p a b", a=8)
                    for kc in range(8):
                        s.tr(pv[:, kc, :], Xe[b][:, c0, kc * 128:(kc + 1) * 128], identb[:],
                             r=["Xe%d_%d" % (b, c0), "identb"], w=["ps%d" % (c0 % 2)])
                    s.op("act", lambda e: e.copy(out=XeT[:, :, c0 * 128:(c0 + 1) * 128], in_=pv), r=["ps%d" % (c0 % 2)], w=["XeT"])
                for fc in range(8):
                    fsl = slice(fc * 128, (fc + 1) * 128)
                    gb_ = 2 + (fc % 2) * 2
                    for kc in range(8):
                        s.mm(PS[gb_][:, :], lhsT=wg[b][:, kc, fsl], rhs=XeT[:, kc, :], start=(kc == 0), stop=(kc == 7),
                             r=["wg%d_%d" % (b, kc // 2), "XeT"], w=["ps%d" % gb_])
                    for kc in range(8):
                        s.mm(PS[gb_ + 1][:, :], lhsT=wu[b][:, kc, fsl], rhs=XeT[:, kc, :], start=(kc == 0), stop=(kc == 7),
                             r=["wu%d_%d" % (b, kc // 2), "XeT"], w=["ps%d" % (gb_ + 1)])
                    s.op("act", lambda e: e.activation(out=sg[:], in_=PS[gb_][:, :], func=AF.Silu), r=["ps%d" % gb_], w=["sg"])
                    s.op("dve", lambda e: e.tensor_tensor(out=hid[:, fc, :], in0=PS[gb_ + 1][:, :], in1=sg[:], op=ALU.mult),
                         r=["ps%d" % (gb_ + 1), "sg"], w=["hid"])
                    if casts:
                        casts.pop(0)()
                for c0 in range(4):
                    yb = yctr % 2
                    yctr += 1
                    gatev = Ge[b][:, c0, :]
                    for half in range(2):
                        bank = 6 + half
                        for fc in range(8):
                            s.mm(PS[bank][:, :], lhsT=hid[:, fc, c0 * 128:(c0 + 1) * 128], rhs=wd[b][:, fc, half * 512:(half + 1) * 512],
                                 start=(fc == 0), stop=(fc == 7), r=["hid", "wd%d_%d" % (b, fc // 2)], w=["ps%d" % bank])
                        s.op("dve", lambda e: e.tensor_scalar(out=ye[yb][:, half * 512:(half + 1) * 512], in0=PS[bank][:, :],
                                                              scalar1=gatev[:, ex:ex + 1], scalar2=None, op0=ALU.mult),
                             r=["ps%d" % bank, "Ge%d_%d" % (b, c0)], w=["ye%d" % yb])
                    s.dma("pool", lambda e: e.indirect_dma_start(out=x1_d[:, :], out_offset=bass.IndirectOffsetOnAxis(ap=idxi[:, ex, c0:c0 + 1], axis=0),
                                                                 in_=ye[yb][:, :], in_offset=None, compute_op=ALU.add),
                          r=["ye%d" % yb, "idxi"] + ["sc%d_%d" % (ex - 1, k) for k in range(4)], w=["sc%d_%d" % (ex, c0)])
                    if casts:
                        casts.pop(0)()
                while casts:
                    casts.pop(0)()
        s.barrier()

        with contextlib.ExitStack() as p5:
            NB5, K5 = 6, 4
            gt = sb("gt5", [128, D], F32, p5)
            xt = [sb("xt5_%d" % i, [128, D], F32, p5) for i in range(NB5)]
            ot = [sb("ot5_%d" % i, [128, D], F32, p5) for i in range(NB5)]
            junk = sb("junk5", [128, D], BF16, p5)
            st = sb("st5", [128, 4 * NB5], F32, p5)
            s.dma("sp", lambda e: e.dma_start(out=gt[:], in_=gfin_d[0:1, :].to_broadcast([128, D])), w=["gt"])

            def load5(i):
                b = i % NB5
                s.dma("sp", lambda e: e.dma_start(out=xt[b][:], in_=x1_d[i * 128:(i + 1) * 128, :]), w=["xt%d" % b])

            for i in range(K5):
                load5(i)
            for i in range(NT):
                b = i % NB5
                if i + K5 < NT:
                    load5(i + K5)
                sq, ms, rs = (st[:, 4 * b + k:4 * b + k + 1] for k in range(3))
                tag = "p5_%d" % b
                s.op("act", lambda e: e.activation(out=junk[:], in_=xt[b][:], func=AF.Square, accum_out=sq),
                     r=["xt%d" % b], w=["junk", tag + "ssq"])
                rstd_from_ssq(sq, ms, rs, D, tag)
                s.op("dve", lambda e: e.scalar_tensor_tensor(out=ot[b][:], in0=xt[b][:], scalar=rs, in1=gt[:],
                                                             op0=ALU.mult, op1=ALU.mult),
                     r=["xt%d" % b, tag + "rstd", "gt"], w=["ot%d" % b])
                s.dma("sp", lambda e: e.dma_start(out=out_d[i * 128:(i + 1) * 128, :], in_=ot[b][:]), r=["ot%d" % b], w=["out%d" % i])
        s.barrier()
    return nc


_NC = None


def kernel(x, norm_mix_g, w_in, b_gate, gmlp_norm_g, w_spatial, b_spatial, w_proj_a, w_proj_b, w_out,
           norm_ffn_g, w_router, w_e_gate, w_e_up, w_e_down, norm_final_g):
    global _NC
    f = lambda a: np.ascontiguousarray(np.asarray(a, dtype=np.float32))
    shared = {
        "norm_mix_g": f(norm_mix_g).reshape(1, D),
        "w_in": f(w_in).reshape(D, INC),
        "b_gate": f(b_gate).reshape(16, 128),
        "gmlp_norm_g": f(gmlp_norm_g).reshape(1, 512),
        "w_spatial": f(w_spatial).reshape(8, 128, 128),
        "b_spatial": f(b_spatial).reshape(8, 128),
        "w_proj_a": f(w_proj_a).reshape(512, D),
        "w_proj_b": f(w_proj_b).reshape(512, D),
        "w_out": f(w_out).reshape(D, D),
        "norm_ffn_g": f(norm_ffn_g).reshape(1, D),
        "w_router": f(w_router).reshape(D, NE),
        "w_e_gate": f(w_e_gate).reshape(NE, D, D),
        "w_e_up": f(w_e_up).reshape(NE, D, D),
        "w_e_down": f(w_e_down).reshape(NE, D, D),
        "norm_final_g": f(norm_final_g).reshape(1, D),
    }
    x = f(x)
    if _NC is None:
        _NC = build()
    in_maps = []
    for c in range(8):
        m = dict(shared)
        m["x"] = x[c % 4]
        in_maps.append(m)
    res = run_bass_kernel_spmd(_NC, in_maps, core_ids=list(range(8)))
    if DEBUG:
        return res
    out = np.stack([np.asarray(res.results[c]["out"], dtype=np.float32).reshape(SEQ, D) for c in range(4)], axis=0)
    return out
```

```python
import contextlib
import numpy as np
import concourse.bass as bass
import concourse.mybir as mybir
from concourse.bass_utils import run_bass_kernel_spmd

F32 = mybir.dt.float32
BF16 = mybir.dt.bfloat16
I32 = mybir.dt.int32
AF = mybir.ActivationFunctionType
ALU = mybir.AluOpType
AX = mybir.AxisListType

SEQ = 4096
D = 1024
NT = SEQ // 128
INC = 4608
NE = 16
CAP = 512
ROW = 1024
EPS = 1e-6
PATTERNS = (1, 4, 16)
NPOOL = 12
SKEW = 2
DEBUG = None


class S:
    def __init__(self, nc, es):
        self.nc = nc
        self.e = {"pe": nc.tensor, "act": nc.scalar, "dve": nc.vector, "pool": nc.gpsimd, "sp": nc.sync}
        self.sem = {}
        for k in ("pe", "act", "dve", "pool"):
            self.sem[k] = es.enter_context(nc.semaphore("s_" + k))
        self.cnt = {k: 0 for k in ("pe", "act", "dve", "pool")}
        self.dsem = {}
        self.duse = {}
        self.drr = {"sp": 0, "pool": 0, "act": 0}
        for q in ("sp", "pool", "act"):
            for i in range(NPOOL):
                self.dsem[(q, i)] = es.enter_context(nc.semaphore("d_%s%d" % (q, i)))
                self.duse[(q, i)] = 0
        self.known = {k: {} for k in self.e}
        self.lw = {}
        self.rd = {}

    def _semh(self, key):
        return self.sem[key] if key in self.sem else self.dsem[key]

    def _wait(self, eng, deps):
        need = {}
        for (k, v) in deps:
            if k == eng:
                if eng == "pe":
                    continue
            if v > need.get(k, 0):
                need[k] = v
        kn = self.known[eng]
        for k, v in need.items():
            if kn.get(k, 0) >= v:
                continue
            self.e[eng].wait_ge(self._semh(k), v)
            kn[k] = v

    def _deps(self, r, w):
        deps = []
        for k in r:
            if k in self.lw:
                deps.append(self.lw[k])
        for k in w:
            if k in self.lw:
                deps.append(self.lw[k])
            deps.extend(self.rd.get(k, {}).items())
        return deps

    def _commit(self, ev, r, w):
        for k in r:
            d = self.rd.setdefault(k, {})
            if ev[1] > d.get(ev[0], 0):
                d[ev[0]] = ev[1]
        for k in w:
            self.lw[k] = ev
            self.rd[k] = {}

    def op(self, eng, fn, r=(), w=()):
        self._wait(eng, self._deps(r, w))
        ins = fn(self.e[eng])
        self.cnt[eng] += 1
        ins.then_inc(self.sem[eng], 1)
        ev = (eng, self.cnt[eng])
        self._commit(ev, r, w)
        return ev

    def mm(self, out, lhsT, rhs, start=True, stop=True, r=(), w=(), skip=False):
        return self.op("pe", lambda e: e.matmul(out, lhsT=lhsT, rhs=rhs, start=start, stop=stop, skip_group_check=skip), r, w)

    def tr(self, out, in_, ident, r=(), w=()):
        return self.op("pe", lambda e: e.transpose(out, in_, ident), r, w)

    def dma(self, q, fn, r=(), w=()):
        i = self.drr[q]
        self.drr[q] = (i + 1) % NPOOL
        key = (q, i)
        deps = self._deps(r, w)
        if self.duse[key] > 0:
            deps.append((key, 16 * self.duse[key]))
        self._wait(q, deps)
        ins = fn(self.e[q])
        self.duse[key] += 1
        ins.then_inc(self.dsem[key], 16)
        ev = (key, 16 * self.duse[key])
        self._commit(ev, r, w)
        return ev

    def barrier(self):
        for eng in ("pe", "act", "dve", "pool", "sp"):
            deps = [(k, self.cnt[k]) for k in self.cnt if k != eng and self.cnt[k] > 0]
            deps += [(k, 16 * u) for k, u in self.duse.items() if u > 0]
            kn = self.known[eng]
            for k, v in deps:
                if kn.get(k, 0) >= v:
                    continue
                self.e[eng].wait_ge(self._semh(k), v)
                kn[k] = v
        self.lw = {}
        self.rd = {}


def tokv(ap, r, d, m0, n):
    if d == 1:
        return ap[:, m0:m0 + n]
    return ap[:, r + d * m0: r + d * (m0 + n - 1) + 1: d]


class _Stop(Exception):
    pass


def build():
    try:
        return _build()
    except _Stop as e:
        return e.args[0]


def _build():
    nc = bass.Bass("TRN2", target_bir_lowering=False)
    dt = nc.dram_tensor
    x_d = dt("x", [SEQ, D], F32, kind="ExternalInput").ap()
    gmix_d = dt("norm_mix_g", [1, D], F32, kind="ExternalInput").ap()
    win_d = dt("w_in", [D, INC], F32, kind="ExternalInput").ap()
    bgate_d = dt("b_gate", [16, 128], F32, kind="ExternalInput").ap()
    ggm_d = dt("gmlp_norm_g", [1, 512], F32, kind="ExternalInput").ap()
    wsp_d = dt("w_spatial", [8, 128, 128], F32, kind="ExternalInput").ap()
    bsp_d = dt("b_spatial", [8, 128], F32, kind="ExternalInput").ap()
    wpa_d = dt("w_proj_a", [512, D], F32, kind="ExternalInput").ap()
    wpb_d = dt("w_proj_b", [512, D], F32, kind="ExternalInput").ap()
    wout_d = dt("w_out", [D, D], F32, kind="ExternalInput").ap()
    gffn_d = dt("norm_ffn_g", [1, D], F32, kind="ExternalInput").ap()
    wr_d = dt("w_router", [D, NE], F32, kind="ExternalInput").ap()
    weg_d = dt("w_e_gate", [NE, D, D], F32, kind="ExternalInput").ap()
    weu_d = dt("w_e_up", [NE, D, D], F32, kind="ExternalInput").ap()
    wed_d = dt("w_e_down", [NE, D, D], F32, kind="ExternalInput").ap()
    gfin_d = dt("norm_final_g", [1, D], F32, kind="ExternalInput").ap()
    out_d = dt("out", [SEQ, D], F32, kind="ExternalOutput").ap()
    hT_d = dt("hT_scr", [128, 8, SEQ], BF16, kind="Internal").ap()
    x1_d = dt("x1_scr", [SEQ, D], F32, kind="Internal").ap()
    h2_d = dt("h2_scr", [SEQ, ROW], BF16, kind="Internal").ap()
    aff_d = dt("aff_scr", [SEQ, NE], F32, kind="Internal").ap()
    dbg_d = None
    if DEBUG:
        dbg_d = dt("dbg", [128, 4 * SEQ], F32, kind="ExternalOutput").ap()

    with contextlib.ExitStack() as es:
        s = S(nc, es)

        def ck(n):
            if DEBUG == "p1:%d" % n:
                s.barrier()
                raise _Stop(nc)

        def sb(name, shape, dtype, stack=es):
            return stack.enter_context(nc.sbuf_tensor(name, shape, dtype))

        PSW = [es.enter_context(nc.psum_tensor("psw%d" % i, [128, 1024], F32)) for i in range(4)]
        PS = [PSW[i // 2][:, (i % 2) * 512:(i % 2 + 1) * 512] for i in range(8)]

        def psb(i):
            return PS[i][:, :].bitcast(BF16)

        identb = sb("identb", [128, 128], BF16)
        identf = sb("identf", [128, 128], F32)
        onesb = sb("onesb", [128, 128], BF16)
        iot = sb("iot", [128, 128], F32)
        nhalf = sb("nhalf", [128, 1], F32)
        idxi = sb("idxi", [128, NE, 4], I32)
        st_aff = contextlib.ExitStack()
        affT = sb("affT", [16, SEQ], F32, st_aff)
        st_oT = contextlib.ExitStack()
        oT = sb("oT", [128, 4, SEQ], BF16, st_oT)

        s.op("pool", lambda e: e.iota(iot[:], pattern=[[1, 128]], base=0, channel_multiplier=-1,
                                      allow_small_or_imprecise_dtypes=True), w=["iot"])
        s.op("dve", lambda e: e.tensor_scalar(out=identf[:], in0=iot[:], scalar1=0.0, scalar2=None, op0=ALU.is_equal),
             r=["iot"], w=["identf"])
        s.op("dve", lambda e: e.tensor_copy(out=identb[:], in_=identf[:]), r=["identf"], w=["identb"])
        s.op("pool", lambda e: e.memset(onesb[:], 1.0), w=["onesb"])
        s.op("pool", lambda e: e.memset(nhalf[:], -0.5), w=["nhalf"])

        def rstd_from_ssq(ssq, ms, rstd, n, tag):
            s.op("dve", lambda e: e.tensor_scalar(out=ms, in0=ssq, scalar1=1.0 / n, scalar2=EPS, op0=ALU.mult, op1=ALU.add),
                 r=[tag + "ssq"], w=[tag + "ms"])
            s.op("pool", lambda e: e.tensor_tensor(out=rstd, in0=ms, in1=nhalf[:, 0:1], op=ALU.pow),
                 r=[tag + "ms", "nhalf"], w=[tag + "rstd"])

        with contextlib.ExitStack() as p0:
            NB0, K0 = 4, 3
            gt = sb("gt0", [128, D], F32, p0)
            xt = [sb("xt0_%d" % i, [128, D], F32, p0) for i in range(NB0)]
            xb = [sb("xb0_%d" % i, [128, D], BF16, p0) for i in range(NB0)]
            junk = sb("junk0", [128, D], BF16, p0)
            st = sb("st0", [128, 4 * NB0], F32, p0)
            hTg = [sb("hTg0_%d" % i, [128, 8, 512], BF16, p0) for i in range(2)]
            s.dma("sp", lambda e: e.dma_start(out=gt[:], in_=gmix_d[0:1, :].to_broadcast([128, D])), w=["gt"])

            def load0(i):
                b = i % NB0
                s.dma("sp", lambda e: e.dma_start(out=xt[b][:], in_=x_d[i * 128:(i + 1) * 128, :]), w=["xt%d" % b])

            def stA0(i):
                b = i % NB0
                if i + K0 < NT:
                    load0(i + K0)
                sq, ms, rs = (st[:, 4 * b + k:4 * b + k + 1] for k in range(3))
                tag = "p0_%d" % b
                s.op("act", lambda e: e.activation(out=junk[:], in_=xt[b][:], func=AF.Square, accum_out=sq),
                     r=["xt%d" % b], w=["junk", tag + "ssq"])
                rstd_from_ssq(sq, ms, rs, D, tag)
                s.op("dve", lambda e: e.scalar_tensor_tensor(out=xb[b][:], in0=xt[b][:], scalar=rs, in1=gt[:],
                                                             op0=ALU.mult, op1=ALU.mult),
                     r=["xt%d" % b, tag + "rstd", "gt"], w=["xb%d" % b])

            def stB0(i):
                b = i % NB0
                g = i // 4
                pv = psb(i % 2).rearrange("p (a b) -> p a b", a=8)
                for kc in range(8):
                    s.tr(pv[:, kc, :], xb[b][:, kc * 128:(kc + 1) * 128], identb[:], r=["xb%d" % b, "identb"], w=["ps%d" % (i % 2)])
                s.op("act", lambda e: e.copy(out=hTg[g % 2][:, :, (i % 4) * 128:(i % 4 + 1) * 128], in_=pv),
                     r=["ps%d" % (i % 2)], w=["hTg%d" % (g % 2)])
                if i % 4 == 3:
                    s.dma("sp", lambda e: e.dma_start(out=hT_d[:, :, g * 512:(g + 1) * 512], in_=hTg[g % 2][:]),
                          r=["hTg%d" % (g % 2)], w=["hT_d%d" % g])

            for i in range(K0):
                load0(i)
            stA0(0)
            stA0(1)
            for i in range(NT):
                stB0(i)
                if i + 2 < NT:
                    stA0(i + 2)
        s.barrier()
        if DEBUG == "p0":
            return nc

        pw = contextlib.ExitStack()
        wuv = sb("wuv", [128, 8, 1024], BF16, pw)
        wpa = sb("wpa", [128, 4, 1024], BF16, pw)
        wpb = sb("wpb", [128, 4, 1024], BF16, pw)
        wo = sb("wo", [128, 8, 1024], BF16, pw)
        with contextlib.ExitStack() as p1:
            wq = sb("wq", [128, 3, 8, 128], BF16, p1)
            hg = [sb("hg1_%d" % i, [128, 8, 512], BF16, p1) for i in range(2)]
            qkv = sb("qkv", [128, 3, SEQ], BF16, p1)
            qT, kT, vT = qkv[:, 0, :], qkv[:, 1, :], qkv[:, 2, :]
            Vl = sb("Vl", [128, 32, 128], BF16, p1)
            PT = [sb("PT%d" % i, [128, 2, 256], BF16, p1) for i in range(4)]
            EX = [sb("EX%d" % i, [128, 2, 256], BF16, p1) for i in range(3)]
            msk = sb("msk", [128, 12, 2, 256], BF16, p1)
            accN = sb("accN", [128, SEQ], F32, p1)
            accD = sb("accD", [128, SEQ], F32, p1)
            scr1 = sb("scr1", [128, 768], F32, p1)
            dlt, band, mtmp = scr1[:, 0:256], scr1[:, 256:512], scr1[:, 512:768]
            dtmpB = sb("dtmpB", [128, 512], F32, p1)
            DT = [scr1[:, 0:512].rearrange("p (a b) -> p a b", a=2), dtmpB[:, :].rearrange("p (a b) -> p a b", a=2)]
            DTK = [["dt0", "dlt", "band"], ["dt1"]]

            s.op("pool", lambda e: e.iota(dlt, pattern=[[-1, 256]], base=64, channel_multiplier=1,
                                          allow_small_or_imprecise_dtypes=True), w=["dlt"])
            s.op("dve", lambda e: e.tensor_scalar(out=band, in0=dlt, scalar1=-1.0, scalar2=None, op0=ALU.mult), r=["dlt"], w=["band"])
            s.op("dve", lambda e: e.tensor_tensor(out=dlt, in0=dlt, in1=band, op=ALU.max), r=["dlt", "band"], w=["dlt"])
            s.op("dve", lambda e: e.tensor_scalar(out=band, in0=dlt, scalar1=64.5, scalar2=None, op0=ALU.is_le),
                 r=["dlt"], w=["band"])
            for pi, dil in enumerate(PATTERNS):
                for hp in range(4):
                    for hh in range(2):
                        h = hp * 2 + hh
                        slope = 2.0 ** (-8.0 * (h + 1) / 8.0)
                        s.op("act", lambda e: e.activation(out=mtmp, in_=dlt, func=AF.Exp, scale=-slope * dil),
                             r=["dlt"], w=["mtmp"])
                        s.op("dve", lambda e: e.tensor_tensor(out=msk[:, pi * 4 + hp, hh, :], in0=mtmp, in1=band, op=ALU.mult),
                             r=["mtmp", "band"], w=["msk"])

            ck(1)
            for hp in range(4):
                for t3 in range(3):
                    c0 = 1024 + 512 * t3 + hp * 128
                    s.dma("pool", lambda e: e.dma_start(out=wq[:, t3, :, :],
                                                        in_=win_d[:, c0:c0 + 128].rearrange("(kc p) c -> p kc c", p=128)),
                          w=["wq"])
                if hp == 0:
                    castw = lambda dst, src, key: s.dma("pool", lambda e: e.dma_start(out=dst, in_=src), w=[key])
                    castw(wuv[:], win_d[:, 0:1024].rearrange("(kc p) c -> p kc c", p=128), "wuv")
                    castw(wpa[:], wpa_d.rearrange("(kc p) c -> p kc c", p=128), "wpa")
                    castw(wpb[:], wpb_d.rearrange("(kc p) c -> p kc c", p=128), "wpb")
                    castw(wo[:], wout_d.rearrange("(kc p) c -> p kc c", p=128), "wo")
                for g in range(8):
                    hb = g % 2
                    s.dma("sp", lambda e: e.dma_start(out=hg[hb][:], in_=hT_d[:, :, g * 512:(g + 1) * 512]),
                          r=["hT_d"], w=["hg%d" % hb])
                    for t3 in range(3):
                        bank = (g * 3 + t3) % 2
                        for kc in range(8):
                            s.mm(PS[bank][:, :], lhsT=wq[:, t3, kc, :], rhs=hg[hb][:, kc, :], start=(kc == 0), stop=(kc == 7),
                                 r=["wq", "hg%d" % hb], w=["ps%d" % bank])
                        s.op("act", lambda e: e.copy(out=qkv[:, t3, g * 512:(g + 1) * 512], in_=PS[bank][:, :]),
                             r=["ps%d" % bank], w=["qkv%d" % t3])
                ck(2)
                s.op("pool", lambda e: e.memset(accN[:], 0.0), w=["accN"])
                s.op("pool", lambda e: e.memset(accD[:], 0.0), w=["accD"])
                blk_ctr = 0
                grp_ctr = 0
                pend = []

                def flushB(keep):
                    while len(pend) > keep:
                        pend.pop(0)()

                def mkA(pi, dil, r, j, L, bc):
                    def fn():
                        qa = max(0, 128 * j - 64)
                        qb_ = min(L, 128 * j + 192)
                        c0 = qa - (128 * j - 64)
                        n = qb_ - qa
                        sw = bc % 2
                        spv = PSW[sw][:, :].rearrange("p (a b) -> p a b", a=2)
                        skeys = ["ps%d" % (2 * sw), "ps%d" % (2 * sw + 1)]
                        for hh in range(2):
                            lo, hi = hh * 64, hh * 64 + 64
                            s.mm(spv[:, hh, c0:c0 + n], lhsT=tokv(kT[lo:hi], r, dil, j * 128, 128),
                                 rhs=tokv(qT[lo:hi], r, dil, qa, n), r=["qkv0", "qkv1"], w=[skeys[hh]])
                        ex = EX[bc % 3]
                        pt = PT[bc % 4]
                        s.op("act", lambda e: e.activation(out=ex[:, :, c0:c0 + n], in_=spv[:, :, c0:c0 + n], func=AF.Exp, scale=0.125),
                             r=skeys, w=["EX%d" % (bc % 3)])
                        s.op("dve", lambda e: e.tensor_tensor(out=pt[:, :, c0:c0 + n], in0=ex[:, :, c0:c0 + n],
                                                               in1=msk[:, pi * 4 + hp, :, c0:c0 + n], op=ALU.mult),
                             r=["EX%d" % (bc % 3), "msk"], w=["PT%d" % (bc % 4)])
                    return fn

                def mkB(pi, dil, r, i, L, nb, sl, gcn):
                    def fn():
                        ma = max(0, 128 * i - 64)
                        mb = min(L, 128 * i + 64)
                        n = mb - ma
                        js = [jj for jj in (i - 1, i) if 0 <= jj < nb]
                        nbank = 4 + gcn % 2
                        dbank = 6 + gcn % 2
                        nv = PS[nbank][:, :].rearrange("p (a b) -> p a b", a=2)
                        dv = PS[dbank][:, :].rearrange("p (a b) -> p a b", a=2)
                        col = (i % 2) * 128 + (ma - (128 * i - 64))
                        for ji, jj in enumerate(js):
                            c = ma - (128 * jj - 64)
                            ptj = PT[sl[jj]]
                            s.mm(nv[:, :, col:col + n], lhsT=Vl[:, r * nb + jj, :], rhs=ptj[:, :, c:c + n],
                                 start=(ji == 0), stop=(ji == len(js) - 1),
                                 r=["Vl", "PT%d" % sl[jj]], w=["ps%d" % nbank])
                        for ji, jj in enumerate(js):
                            c = ma - (128 * jj - 64)
                            ptj = PT[sl[jj]]
                            s.mm(dv[:, :, col:col + n], lhsT=onesb[:], rhs=ptj[:, :, c:c + n],
                                 start=(ji == 0), stop=(ji == len(js) - 1),
                                 r=["onesb", "PT%d" % sl[jj]], w=["ps%d" % dbank])
                        if i % 2 == 1 or i == nb:
                            g = i // 2
                            ga = max(0, 256 * g - 64)
                            gb_ = min(L, 256 * g + 192)
                            gn = gb_ - ga
                            gc = ga - (256 * g - 64)
                            dtv = DT[gcn % 2]
                            dtk = DTK[gcn % 2]
                            s.op("act", lambda e: e.copy(out=dtv[:, :, gc:gc + gn], in_=dv[:, :, gc:gc + gn]),
                                 r=["ps%d" % dbank], w=dtk)
                            for hh in range(2):
                                lo, hi = hh * 64, hh * 64 + 64
                                av = tokv(accN[lo:hi], r, dil, ga, gn)
                                s.op("dve", lambda e: e.tensor_tensor(out=av, in0=av, in1=nv[lo:hi, hh, gc:gc + gn], op=ALU.add),
                                     r=["ps%d" % nbank, "accN"], w=["accN"])
                                ad = tokv(accD[lo:hi], r, dil, ga, gn)
                                s.op("pool", lambda e: e.tensor_tensor(out=ad, in0=ad, in1=dtv[lo:hi, hh, gc:gc + gn], op=ALU.add),
                                     r=[dtk[0], "accD"], w=["accD"])
                    return fn

                for pi, dil in enumerate(PATTERNS):
                    L = SEQ // dil
                    nb = L // 128
                    flushB(0)
                    for r in range(dil):
                        for j in range(nb):
                            blk = r * nb + j
                            pvw = psb(2).rearrange("p (a b) -> p a b", a=8)
                            s.tr(pvw[:, blk % 8, :], tokv(vT, r, dil, j * 128, 128), identb[:], r=["qkv2", "identb"], w=["ps2"])
                            if blk % 8 == 7:
                                s.op("act", lambda e: e.copy(out=Vl[:, blk - 7:blk + 1, :], in_=pvw), r=["ps2"], w=["Vl"])
                    for r in range(dil):
                        slots = {}
                        for j in range(nb + 1):
                            if j < nb:
                                mkA(pi, dil, r, j, L, blk_ctr)()
                                slots[j] = blk_ctr % 4
                                blk_ctr += 1
                            pend.append(mkB(pi, dil, r, j, L, nb, dict(slots), grp_ctr))
                            if j % 2 == 1 or j == nb:
                                grp_ctr += 1
                            flushB(SKEW)
                flushB(0)
                for hf in range(2):
                    sl = slice(hf * 2048, (hf + 1) * 2048)
                    s.op("dve", lambda e: e.reciprocal(out=accD[:, sl], in_=accD[:, sl]), r=["accD"], w=["accD"])
                    s.op("dve", lambda e: e.tensor_tensor(out=oT[:, hp, sl], in0=accN[:, sl], in1=accD[:, sl], op=ALU.mult),
                         r=["accN", "accD"], w=["oT"])
        s.barrier()
        if DEBUG == "oT":
            with contextlib.ExitStack() as pd:
                tmpf = sb("dbgtmp", [128, SEQ], F32, pd)
                for hp in range(4):
                    s.op("dve", lambda e: e.tensor_copy(out=tmpf[:], in_=oT[:, hp, :]), r=["oT"], w=["tmpf"])
                    s.dma("sp", lambda e: e.dma_start(out=dbg_d[:, hp * SEQ:(hp + 1) * SEQ], in_=tmpf[:]), r=["tmpf"], w=["dbg"])
                s.barrier()
            return nc

        with contextlib.ExitStack() as p2:
            wgg = sb("wgg", [128, 8, 2048], BF16, p2)
            wrt = sb("wrt", [128, 8, NE], BF16, p2)
            wsf = sb("wsf", [128, 8, 128], F32, p2)
            wsT = sb("wsT", [128, 8, 128], BF16, p2)
            bspr = sb("bspr", [8, 128], F32, p2)
            bspt = sb("bspt", [128, 8], F32, p2)
            bg = sb("bg", [128, 16], F32, p2)
            bgr = sb("bgr", [16, 128], F32, p2)
            ggt = sb("ggt", [128, 512], F32, p2)
            g2t = sb("g2t", [128, D], F32, p2)
            hg = [sb("hg2_%d" % i, [128, 8, 512], BF16, p2) for i in range(2)]
            gu = sb("gu", [128, 512], BF16, p2)
            gv = sb("gv", [128, 512], F32, p2)
            vn = sb("vn", [128, 512], BF16, p2)
            ab = sb("ab", [128, 512], BF16, p2)
            aT = [sb("aT%d" % i, [128, 4, 512], BF16, p2) for i in range(2)]
            sga = sb("sga", [128, 512], F32, p2)
            sgb = sb("sgb", [128, 512], F32, p2)
            t1 = sga
            t2 = sgb
            mT = [sb("mT%d" % i, [128, 8, 512], BF16, p2) for i in range(2)]
            x1 = [sb("x1_%d" % i, [128, D], F32, p2) for i in range(2)]
            hrow = [sb("hrow%d" % i, [128, ROW], BF16, p2) for i in range(2)]
            h2T = sb("h2T", [128, 8, 128], BF16, p2)
            st = sb("st2", [128, 16], F32, p2)
            lg = sb("lg", [128, NE], F32, p2)
            aff = sb("aff", [128, NE], F32, p2)

            cast = lambda dst, src, key: s.dma("pool", lambda e: e.dma_start(out=dst, in_=src), w=[key])
            s.dma("sp", lambda e: e.dma_start(out=wsf[:], in_=wsp_d.rearrange("g t s -> t g s")), w=["wsf"])
            s.dma("sp", lambda e: e.dma_start(out=bspr[:], in_=bsp_d), w=["bspr"])
            s.dma("sp", lambda e: e.dma_start(out=bgr[:], in_=bgate_d), w=["bgr"])
            s.dma("sp", lambda e: e.dma_start(out=ggt[:], in_=ggm_d[0:1, :].to_broadcast([128, 512])), w=["ggt"])
            s.dma("sp", lambda e: e.dma_start(out=g2t[:], in_=gffn_d[0:1, :].to_broadcast([128, D])), w=["g2t"])
            cast(wgg[:], win_d[:, 2560:4608].rearrange("(kc p) c -> p kc c", p=128), "wgg")
            cast(wrt[:], wr_d.rearrange("(kc p) c -> p kc c", p=128), "wrt")
            for g in range(8):
                s.tr(PS[0][:, 0:128], wsf[:, g, :], identf[:], r=["wsf", "identf"], w=["ps0"])
                s.op("dve", lambda e: e.tensor_copy(out=wsT[:, g, :], in_=PS[0][:, 0:128]), r=["ps0"], w=["wsT"])
            s.tr(PS[0][:, 0:8], bspr[:, :], identf[0:8, 0:8], r=["bspr", "identf"], w=["ps0"])
            s.op("dve", lambda e: e.tensor_copy(out=bspt[:], in_=PS[0][:, 0:8]), r=["ps0"], w=["bspt"])
            s.tr(PS[0][:, 0:16], bgr[:, :], identf[0:16, 0:16], r=["bgr", "identf"], w=["ps0"])
            s.op("dve", lambda e: e.tensor_copy(out=bg[:], in_=PS[0][:, 0:16]), r=["ps0"], w=["bg"])

            def Ia(g, t):
                hb = g % 2
                tsl = slice(t * 128, (t + 1) * 128)
                if t == 0:
                    s.dma("act", lambda e: e.dma_start(out=hg[hb][:], in_=hT_d[:, :, g * 512:(g + 1) * 512]), r=["hT_d"], w=["hg%d" % hb])
                for half in range(2):
                    for kc in range(8):
                        s.mm(PS[half][:, :], lhsT=hg[hb][:, kc, tsl], rhs=wuv[:, kc, half * 512:(half + 1) * 512],
                             start=(kc == 0), stop=(kc == 7), r=["hg%d" % hb, "wuv"], w=["ps%d" % half])
                s.op("act", lambda e: e.activation(out=gu[:], in_=PS[0][:, :], func=AF.Gelu_apprx_tanh), r=["ps0"], w=["gu"])
                s.op("act", lambda e: e.activation(out=gv[:], in_=PS[1][:, :], func=AF.Gelu_apprx_tanh), r=["ps1"], w=["gv"])
                s.op("act", lambda e: e.activation(out=vn[:], in_=gv[:], func=AF.Square, accum_out=st[:, 0:1]),
                     r=["gv"], w=["vn", "gmssq"])
                rstd_from_ssq(st[:, 0:1], st[:, 1:2], st[:, 2:3], 512, "gm")
                s.op("dve", lambda e: e.scalar_tensor_tensor(out=vn[:], in0=gv[:], scalar=st[:, 2:3], in1=ggt[:],
                                                             op0=ALU.mult, op1=ALU.mult),
                     r=["gv", "gmrstd", "ggt"], w=["vn"])

            def Ib(g, t):
                for gg in range(8):
                    s.mm(PS[2][:, gg * 64:(gg + 1) * 64], lhsT=wsT[:, gg, :], rhs=vn[:, gg * 64:(gg + 1) * 64],
                         r=["wsT", "vn"], w=["ps2"])
                s.op("dve", lambda e: e.tensor_tensor(out=gv[:].rearrange("p (a b) -> p a b", a=8),
                                                      in0=PS[2][:, :].rearrange("p (a b) -> p a b", a=8),
                                                      in1=bspt[:].unsqueeze(2).to_broadcast([128, 8, 64]), op=ALU.add),
                     r=["ps2", "bspt"], w=["gv"])
                s.op("dve", lambda e: e.tensor_tensor(out=ab[:], in0=gv[:], in1=gu[:], op=ALU.mult), r=["gv", "gu"], w=["ab"])

            def Ic(g, t):
                tsl = slice(t * 128, (t + 1) * 128)
                pv = psb(3).rearrange("p (a b) -> p a b", a=8)
                for kc in range(4):
                    s.tr(pv[:, kc, :], ab[:, kc * 128:(kc + 1) * 128], identb[:], r=["ab", "identb"], w=["ps3"])
                s.op("act", lambda e: e.copy(out=aT[g % 2][:, :, tsl], in_=pv[:, 0:4, :]), r=["ps3"], w=["aT%d" % (g % 2)])

            def II(g, c):
                hb = g % 2
                gsl = slice(g * 512, (g + 1) * 512)
                csl = slice(c * 128, (c + 1) * 128)
                aTg = aT[g % 2]
                for kc in range(8):
                    s.mm(PS[4][:, :], lhsT=wgg[:, kc, csl], rhs=hg[hb][:, kc, :], start=(kc == 0), stop=(kc == 7),
                         r=["wgg", "hg%d" % hb], w=["ps4"])
                for kc in range(8):
                    s.mm(PS[5][:, :], lhsT=wgg[:, kc, 1024 + c * 128:1024 + (c + 1) * 128], rhs=hg[hb][:, kc, :],
                         start=(kc == 0), stop=(kc == 7), r=["wgg", "hg%d" % hb], w=["ps5"])
                for kc in range(4):
                    s.mm(PS[6][:, :], lhsT=wpa[:, kc, csl], rhs=aTg[:, kc, :], start=(kc == 0), stop=(kc == 3),
                         r=["wpa", "aT%d" % (g % 2)], w=["ps6"])
                for kc in range(4):
                    s.mm(PS[7][:, :], lhsT=wpb[:, kc, csl], rhs=oT[:, kc, gsl], start=(kc == 0), stop=(kc == 3),
                         r=["wpb", "oT"], w=["ps7"])
                s.op("act", lambda e: e.activation(out=sga[:], in_=PS[4][:, :], func=AF.Sigmoid, bias=bg[:, c:c + 1], scale=1.0),
                     r=["ps4", "bg"], w=["sga"])
                s.op("act", lambda e: e.activation(out=sgb[:], in_=PS[5][:, :], func=AF.Sigmoid, bias=bg[:, 8 + c:9 + c], scale=1.0),
                     r=["ps5", "bg"], w=["sgb"])
                s.op("dve", lambda e: e.tensor_tensor(out=t1[:], in0=PS[6][:, :], in1=sga[:], op=ALU.mult), r=["ps6", "sga"], w=["sga"])
                s.op("dve", lambda e: e.tensor_tensor(out=t2[:], in0=PS[7][:, :], in1=sgb[:], op=ALU.mult), r=["ps7", "sgb"], w=["sgb"])
                s.op("pool", lambda e: e.tensor_tensor(out=mT[g % 2][:, c, :], in0=t1[:], in1=t2[:], op=ALU.add),
                     r=["sga", "sgb"], w=["mT%d" % (g % 2)])

            def IIIa(g, t):
                i = g * 4 + t
                b = i % 2
                tsl = slice(t * 128, (t + 1) * 128)
                s.dma("act", lambda e: e.dma_start(out=x1[b][:], in_=x_d[i * 128:(i + 1) * 128, :]), w=["x1_%d" % b])
                for half in range(2):
                    for kc in range(8):
                        s.mm(PS[half][:, :], lhsT=mT[g % 2][:, kc, tsl], rhs=wo[:, kc, half * 512:(half + 1) * 512],
                             start=(kc == 0), stop=(kc == 7), r=["mT%d" % (g % 2), "wo"], w=["ps%d" % half])
                    hs = slice(half * 512, (half + 1) * 512)
                    s.op("dve", lambda e: e.tensor_tensor(out=x1[b][:, hs], in0=PS[half][:, :], in1=x1[b][:, hs], op=ALU.add),
                         r=["ps%d" % half, "x1_%d" % b], w=["x1_%d" % b])
                s.dma("sp", lambda e: e.dma_start(out=x1_d[i * 128:(i + 1) * 128, :], in_=x1[b][:]), r=["x1_%d" % b], w=["x1_d%d" % i])
                s.op("act", lambda e: e.activation(out=hrow[b][:, 0:D], in_=x1[b][:], func=AF.Square, accum_out=st[:, 4:5]),
                     r=["x1_%d" % b], w=["hrow%d" % b, "rtssq"])
                rstd_from_ssq(st[:, 4:5], st[:, 5:6], st[:, 6:7], D, "rt")
                s.op("dve", lambda e: e.scalar_tensor_tensor(out=hrow[b][:, 0:D], in0=x1[b][:], scalar=st[:, 6:7], in1=g2t[:],
                                                             op0=ALU.mult, op1=ALU.mult),
                     r=["x1_%d" % b, "rtrstd", "g2t"], w=["hrow%d" % b])

            def IIIb1(g, t):
                i = g * 4 + t
                b = i % 2
                pv = psb(3).rearrange("p (a b) -> p a b", a=8)
                for kc in range(8):
                    s.tr(pv[:, kc, :], hrow[b][:, kc * 128:(kc + 1) * 128], identb[:], r=["hrow%d" % b, "identb"], w=["ps3"])
                s.op("act", lambda e: e.copy(out=h2T[:], in_=pv), r=["ps3"], w=["h2T"])
                s.dma("sp", lambda e: e.dma_start(out=h2_d[i * 128:(i + 1) * 128, :], in_=hrow[b][:]), r=["hrow%d" % b], w=["h2_d%d" % i])

            def IIIb2(g, t):
                i = g * 4 + t
                for kc in range(8):
                    s.mm(PS[2][:, 0:NE], lhsT=h2T[:, kc, :], rhs=wrt[:, kc, :], start=(kc == 0), stop=(kc == 7),
                         r=["h2T", "wrt"], w=["ps2"])
                s.op("dve", lambda e: e.tensor_reduce(out=st[:, 8:9], in_=PS[2][:, 0:NE], axis=AX.X, op=ALU.max), r=["ps2"], w=["smx"])
                s.op("dve", lambda e: e.tensor_scalar(out=st[:, 9:10], in0=st[:, 8:9], scalar1=-1.0, scalar2=None, op0=ALU.mult),
                     r=["smx"], w=["snmx"])
                s.op("act", lambda e: e.activation(out=lg[:], in_=PS[2][:, 0:NE], func=AF.Exp, bias=st[:, 9:10], scale=1.0,
                                                   accum_out=st[:, 10:11]),
                     r=["ps2", "snmx"], w=["lg", "ssum"])
                s.op("dve", lambda e: e.reciprocal(out=st[:, 11:12], in_=st[:, 10:11]), r=["ssum"], w=["srs"])
                s.op("dve", lambda e: e.tensor_scalar(out=aff[:], in0=lg[:], scalar1=st[:, 11:12], scalar2=None, op0=ALU.mult),
                     r=["lg", "srs"], w=["aff"])
                s.dma("sp", lambda e: e.dma_start(out=aff_d[i * 128:(i + 1) * 128, :], in_=aff[:]), r=["aff"], w=["aff_d%d" % i])

            def IIIc(g, t):
                i = g * 4 + t
                s.tr(PS[2][0:16, 128:256], aff[:, :], identf[:], r=["aff", "identf"], w=["ps2"])
                s.op("act", lambda e: e.copy(out=affT[:, i * 128:(i + 1) * 128], in_=PS[2][0:16, 128:256]), r=["ps2"], w=["affT"])

            NSLOT = 8 * 10 + 12
            pre = [[] for _ in range(NSLOT)]
            mid = [[] for _ in range(NSLOT)]
            post = [[] for _ in range(NSLOT)]
            last = [[] for _ in range(NSLOT)]
            mk = lambda f, g, t: (lambda: f(g, t))
            for g in range(8):
                for t in range(4):
                    b0 = 8 * g
                    last[b0 + 2 * t].append(mk(Ia, g, t))
                    post[b0 + 2 * t + 1].append(mk(Ib, g, t))
                    pre[b0 + 2 * t + 2].append(mk(Ic, g, t))
                    b2 = 8 * (g + 2)
                    last[b2 + 2 * t + 1].append(mk(IIIa, g, t))
                    post[b2 + 2 * t + 2].append(mk(IIIb1, g, t))
                    pre[b2 + 2 * t + 3].append(mk(IIIb2, g, t))
                    post[b2 + 2 * t + 3].append(mk(IIIc, g, t))
                for c in range(8):
                    mid[8 * (g + 1) + c].append(mk(II, g, c))
            for sl in range(NSLOT):
                for lst in (pre[sl], mid[sl], post[sl], last[sl]):
                    for fn in lst:
                        fn()
        pw.close()
        st_oT.close()
        s.barrier()

        p4a = contextlib.ExitStack()
        NSTG = 6
        stg = [sb("stg%d" % i, [128, 2, D], F32, p4a) for i in range(NSTG)]
        wg = [sb("wg0", [128, 8, D], BF16, p4a), None]
        wu = [sb("wu0", [128, 8, D], BF16, p4a), None]
        wd = [sb("wd0", [128, 8, D], BF16, p4a), None]
        chunks = [(ex_, mi, q) for ex_ in range(NE) for mi in range(3) for q in range(4)]
        mats = ((wg, weg_d, "wg"), (wu, weu_d, "wu"), (wd, wed_d, "wd"))

        def emit_dma(n):
            ex_, mi, q = chunks[n]
            k = n % NSTG
            src = mats[mi][1]
            s.dma("sp", lambda e: e.dma_start(out=stg[k][:], in_=src[ex_, q * 256:(q + 1) * 256, :].rearrange("(kc p) c -> p kc c", p=128)),
                  w=["stg%d" % k])

        def emit_cast(n):
            ex_, mi, q = chunks[n]
            k = n % NSTG
            b_ = ex_ % 2
            dst = mats[mi][0][b_]
            wkey = "%s%d_%d" % (mats[mi][2], b_, q)
            if n % 2 == 0 or n < 12:
                s.op("act", lambda e: e.copy(out=dst[:, 2 * q:2 * q + 2, :], in_=stg[k][:]), r=["stg%d" % k], w=[wkey])
            else:
                s.op("dve", lambda e: e.tensor_copy(out=dst[:, 2 * q:2 * q + 2, :], in_=stg[k][:]), r=["stg%d" % k], w=[wkey])
            if n + NSTG < len(chunks):
                emit_dma(n + NSTG)

        def load_w(ex_):
            return [(lambda n=n: emit_cast(n)) for n in range(12 * ex_, 12 * ex_ + 12)]

        for n in range(NSTG):
            emit_dma(n)
        for fn in load_w(0):
            fn()

        with contextlib.ExitStack() as p3:
            cmp_ = sb("cmp", [16, SEQ], F32, p3)
            ones16 = sb("ones16", [16, SEQ], F32, p3)
            cs = sb("cs", [16, SEQ], F32, p3)
            bs = sb("bs", [16, 8], F32, p3)
            csT = sb("csT", [128, NT * NE], F32, p3)
            csI = sb("csI", [128, NT * NE], I32, p3)
            aI = sb("aI", [128, NT * NE], I32, p3)
            bI = sb("bI", [128, NT * NE], I32, p3)
            aF = sb("aF", [128, NT, NE], BF16, p3)
            bF = sb("bF", [128, NT, NE], F32, p3)
            io4 = sb("io4", [128, 4], F32, p3)
            ones4 = sb("ones4", [128, NE, 4], BF16, p3)
            EQ = [sb("EQ%d" % i, [128, NE, 128], BF16, p3) for i in range(2)]
            LT = [sb("LT%d" % i, [128, NE, 128], BF16, p3) for i in range(2)]
            LEb = [sb("LEb%d" % i, [128, NE, 4], BF16, p3) for i in range(2)]
            idxf = sb("idxf", [128, NE * 4], F32, p3)
            lo, hi, mid, cnt, ge, dd = (bs[:, k:k + 1] for k in range(6))
            s.op("dve", lambda e: e.memset(bs[:, 0:1], 0.0), w=["lo"])
            s.op("dve", lambda e: e.memset(bs[:, 1:2], 1.0), w=["hi"])
            s.op("pool", lambda e: e.memset(ones16[:], 1.0), w=["ones16"])
            s.op("pool", lambda e: e.memset(ones4[:], 1.0), w=["ones4"])
            s.op("pool", lambda e: e.iota(io4[:], pattern=[[1, 4]], base=0, channel_multiplier=0,
                                          allow_small_or_imprecise_dtypes=True), w=["io4"])
            for it in range(30):
                wk = 0.5 ** (it + 1)
                s.op("dve", lambda e: e.tensor_scalar(out=mid, in0=lo, scalar1=wk, scalar2=None, op0=ALU.add), r=["lo"], w=["mid"])
                s.op("dve", lambda e: e.tensor_scalar(out=cmp_[:], in0=affT[:], scalar1=mid, scalar2=0.0, op0=ALU.is_ge, op1=ALU.add,
                                                      accum_out=cnt),
                     r=["affT", "mid"], w=["cmp", "cnt"])
                s.op("dve", lambda e: e.tensor_scalar(out=ge, in0=cnt, scalar1=CAP - 0.5, scalar2=None, op0=ALU.is_ge), r=["cnt"], w=["ge"])
                s.op("dve", lambda e: e.scalar_tensor_tensor(out=lo, in0=ge, scalar=wk, in1=lo, op0=ALU.mult, op1=ALU.add),
                     r=["ge", "lo"], w=["lo"])
            s.op("dve", lambda e: e.tensor_scalar(out=cmp_[:], in0=affT[:], scalar1=lo, scalar2=None, op0=ALU.is_ge), r=["affT", "lo"], w=["cmp"])
            s.op("dve", lambda e: e.tensor_tensor_scan(out=cs[:], data0=ones16[:], data1=cmp_[:], initial=0.0, op0=ALU.mult, op1=ALU.add),
                 r=["cmp", "ones16"], w=["cs"])
            for i in range(NT):
                s.tr(PS[0][:, i * NE:(i + 1) * NE], cs[:, i * 128:(i + 1) * 128], identf[0:16, 0:16], r=["cs", "identf"], w=["ps0"])
            s.op("dve", lambda e: e.tensor_copy(out=csT[:], in_=PS[0][:, :]), r=["ps0"], w=["csT"])
            s.op("dve", lambda e: e.tensor_copy(out=csI[:], in_=csT[:]), r=["csT"], w=["csI"])
            s.op("dve", lambda e: e.tensor_single_scalar(out=aI[:], in_=csI[:], scalar=2, op=ALU.arith_shift_right), r=["csI"], w=["aI"])
            s.op("dve", lambda e: e.tensor_single_scalar(out=bI[:], in_=csI[:], scalar=3, op=ALU.bitwise_and), r=["csI"], w=["bI"])
            s.op("dve", lambda e: e.tensor_copy(out=aF[:].rearrange("p a b -> p (a b)"), in_=aI[:]), r=["aI"], w=["aF"])
            s.op("dve", lambda e: e.tensor_copy(out=bF[:].rearrange("p a b -> p (a b)"), in_=bI[:]), r=["bI"], w=["bF"])
            ioc = sb("ioc", [128, 128], F32, p3)
            s.op("pool", lambda e: e.iota(ioc[:], pattern=[[1, 128]], base=0, channel_multiplier=0,
                                          allow_small_or_imprecise_dtypes=True), w=["ioc"])
            iocb = sb("iocb", [128, 128], BF16, p3)
            s.op("dve", lambda e: e.tensor_copy(out=iocb[:], in_=ioc[:]), r=["ioc"], w=["iocb"])
            idv = PS[1][:, 0:NE * 4].rearrange("p (a b) -> p a b", a=NE)
            for i in range(NT):
                b = i % 2
                s.op("dve", lambda e: e.tensor_tensor(out=EQ[b][:], in0=aF[:, i, :].unsqueeze(2).to_broadcast([128, NE, 128]),
                                                      in1=iocb[:].unsqueeze(1).to_broadcast([128, NE, 128]), op=ALU.is_equal),
                     r=["aF", "iocb"], w=["EQ%d" % b])
                s.op("dve", lambda e: e.tensor_tensor(out=LT[b][:], in0=aF[:, i, :].unsqueeze(2).to_broadcast([128, NE, 128]),
                                                       in1=iocb[:].unsqueeze(1).to_broadcast([128, NE, 128]), op=ALU.is_lt),
                     r=["aF", "iocb"], w=["LT%d" % b])
                s.op("dve", lambda e: e.tensor_tensor(out=LEb[b][:], in0=bF[:, i, :].unsqueeze(2).to_broadcast([128, NE, 4]),
                                                      in1=io4[:].unsqueeze(1).to_broadcast([128, NE, 4]), op=ALU.is_le),
                     r=["bF", "io4"], w=["LEb%d" % b])
                for ex in range(NE):
                    s.mm(idv[:, ex, :], lhsT=EQ[b][:, ex, :], rhs=LEb[b][:, ex, :], start=(i == 0 and ex == 0), stop=False,
                         r=["EQ%d" % b, "LEb%d" % b], w=["ps1"], skip=True)
                    s.mm(idv[:, ex, :], lhsT=LT[b][:, ex, :], rhs=ones4[:, ex, :], start=False, stop=(i == NT - 1),
                         r=["LT%d" % b, "ones4"], w=["ps1"], skip=True)
            s.op("dve", lambda e: e.tensor_copy(out=idxf[:], in_=PS[1][:, 0:NE * 4]), r=["ps1"], w=["idxf"])
            s.op("dve", lambda e: e.tensor_copy(out=idxi[:].rearrange("p a b -> p (a b)"), in_=idxf[:]), r=["idxf"], w=["idxi"])
        s.barrier()
        if DEBUG == "idx":
            with contextlib.ExitStack() as pd:
                tmpf = sb("dbgtmp", [128, SEQ], F32, pd)
                s.op("pool", lambda e: e.memset(tmpf[:], 0.0), w=["tmpf"])
                s.op("dve", lambda e: e.tensor_copy(out=tmpf[:, 0:64], in_=idxi[:].rearrange("p a b -> p (a b)")), r=["idxi"], w=["tmpf"])
                s.dma("sp", lambda e: e.dma_start(out=dbg_d[:, 0:SEQ], in_=tmpf[:]), r=["tmpf"], w=["dbg"])
                s.op("dve", lambda e: e.tensor_copy(out=tmpf[0:16, :], in_=affT[:]), r=["affT"], w=["tmpf"])
                s.dma("sp", lambda e: e.dma_start(out=dbg_d[:, SEQ:2 * SEQ], in_=tmpf[:]), r=["tmpf"], w=["dbg"])
                s.barrier()
            return nc

        with contextlib.ExitStack() as p4:
            wg[1] = sb("wg1", [128, 8, D], BF16, p4)
            wu[1] = sb("wu1", [128, 8, D], BF16, p4)
            wd[1] = sb("wd1", [128, 8, D], BF16, p4)
            Xe = [sb("Xe%d" % i, [128, 4, ROW], BF16, p4) for i in range(2)]
            XeT = sb("XeT", [128, 8, CAP], BF16, p4)
            hid = sb("hid", [128, 8, CAP], BF16, p4)
            sg = sb("sg", [128, CAP], F32, p4)
            ye = [sb("ye%d" % i, [128, D], F32, p4) for i in range(2)]
            Ge = [sb("Ge%d" % i, [128, 4, NE], F32, p4) for i in range(2)]

            def gather(ex):
                b = ex % 2
                for c0 in range(4):
                    s.dma("pool", lambda e: e.indirect_dma_start(out=Xe[b][:, c0, :], out_offset=None, in_=h2_d[:, :],
                                                                 in_offset=bass.IndirectOffsetOnAxis(ap=idxi[:, ex, c0:c0 + 1], axis=0)),
                          r=["idxi", "h2_d"], w=["Xe%d_%d" % (b, c0)])
                    s.dma("pool", lambda e: e.indirect_dma_start(out=Ge[b][:, c0, :], out_offset=None, in_=aff_d[:, :],
                                                                 in_offset=bass.IndirectOffsetOnAxis(ap=idxi[:, ex, c0:c0 + 1], axis=0)),
                          r=["idxi", "aff_d"], w=["Ge%d_%d" % (b, c0)])

            gather(0)
            yctr = 0
            for ex in range(NE):
                b = ex % 2
                casts = []
                if ex + 1 < NE:
                    casts = load_w(ex + 1)
                    gather(ex + 1)
                for c0 in range(4):
                    pv = psb(c0 % 2).rearrange("p (a b) -> p a b", a=8)
                    for kc in range(8):
                        s.tr(pv[:, kc, :], Xe[b][:, c0, kc * 128:(kc + 1) * 128], identb[:],
                             r=["Xe%d_%d" % (b, c0), "identb"], w=["ps%d" % (c0 % 2)])
                    s.op("act", lambda e: e.copy(out=XeT[:, :, c0 * 128:(c0 + 1) * 128], in_=pv), r=["ps%d" % (c0 % 2)], w=["XeT"])
                for fc in range(8):
                    fsl = slice(fc * 128, (fc + 1) * 128)
                    gb_ = 2 + (fc % 2) * 2
                    for kc in range(8):
                        s.mm(PS[gb_][:, :], lhsT=wg[b][:, kc, fsl], rhs=XeT[:, kc, :], start=(kc == 0), stop=(kc == 7),
                             r=["wg%d_%d" % (b, kc // 2), "XeT"], w=["ps%d" % gb_])
                    for kc in range(8):
                        s.mm(PS[gb_ + 1][:, :], lhsT=wu[b][:, kc, fsl], rhs=XeT[:, kc, :], start=(kc == 0), stop=(kc == 7),
                             r=["wu%d_%d" % (b, kc // 2), "XeT"], w=["ps%d" % (gb_ + 1)])
                    s.op("act", lambda e: e.activation(out=sg[:], in_=PS[gb_][:, :], func=AF.Silu), r=["ps%d" % gb_], w=["sg"])
                    s.op("dve", lambda e: e.tensor_tensor(out=hid[:, fc, :], in0=PS[gb_ + 1][:, :], in1=sg[:], op=ALU.mult),
                         r=["ps%d" % (gb_ + 1), "sg"], w=["hid"])
                    if casts:
                        casts.pop(0)()
                for c0 in range(4):
                    yb = yctr % 2
                    yctr += 1
                    gatev = Ge[b][:, c0, :]
                    for half in range(2):
                        bank = 6 + half
                        for fc in range(8):
                            s.mm(PS[bank][:, :], lhsT=hid[:, fc, c0 * 128:(c0 + 1) * 128], rhs=wd[b][:, fc, half * 512:(half + 1) * 512],
                                 start=(fc == 0), stop=(fc == 7), r=["hid", "wd%d_%d" % (b, fc // 2)], w=["ps%d" % bank])
                        s.op("dve", lambda e: e.tensor_scalar(out=ye[yb][:, half * 512:(half + 1) * 512], in0=PS[bank][:, :],
                                                              scalar1=gatev[:, ex:ex + 1], scalar2=None, op0=ALU.mult),
                             r=["ps%d" % bank, "Ge%d_%d" % (b, c0)], w=["ye%d" % yb])
                    s.dma("pool", lambda e: e.indirect_dma_start(out=x1_d[:, :], out_offset=bass.IndirectOffsetOnAxis(ap=idxi[:, ex, c0:c0 + 1], axis=0),
                                                                 in_=ye[yb][:, :], in_offset=None, compute_op=ALU.add),
                          r=["ye%d" % yb, "idxi"] + ["sc%d_%d" % (ex - 1, k) for k in range(4)], w=["sc%d_%d" % (ex, c0)])
                    if casts:
                        casts.pop(0)()
                while casts:
                    casts.pop(0)()
        p4a.close()
        st_aff.close()
        s.barrier()

        with contextlib.ExitStack() as p5:
            NB5, K5 = 6, 4
            gt = sb("gt5", [128, D], F32, p5)
            xt = [sb("xt5_%d" % i, [128, D], F32, p5) for i in range(NB5)]
            ot = [sb("ot5_%d" % i, [128, D], F32, p5) for i in range(NB5)]
            junk = sb("junk5", [128, D], BF16, p5)
            st = sb("st5", [128, 4 * NB5], F32, p5)
            s.dma("sp", lambda e: e.dma_start(out=gt[:], in_=gfin_d[0:1, :].to_broadcast([128, D])), w=["gt"])

            def load5(i):
                b = i % NB5
                s.dma("sp", lambda e: e.dma_start(out=xt[b][:], in_=x1_d[i * 128:(i + 1) * 128, :]), w=["xt%d" % b])

            for i in range(K5):
                load5(i)
            for i in range(NT):
                b = i % NB5
                if i + K5 < NT:
                    load5(i + K5)
                sq, ms, rs = (st[:, 4 * b + k:4 * b + k + 1] for k in range(3))
                tag = "p5_%d" % b
                s.op("act", lambda e: e.activation(out=junk[:], in_=xt[b][:], func=AF.Square, accum_out=sq),
                     r=["xt%d" % b], w=["junk", tag + "ssq"])
                rstd_from_ssq(sq, ms, rs, D, tag)
                s.op("dve", lambda e: e.scalar_tensor_tensor(out=ot[b][:], in0=xt[b][:], scalar=rs, in1=gt[:],
                                                             op0=ALU.mult, op1=ALU.mult),
                     r=["xt%d" % b, tag + "rstd", "gt"], w=["ot%d" % b])
                s.dma("sp", lambda e: e.dma_start(out=out_d[i * 128:(i + 1) * 128, :], in_=ot[b][:]), r=["ot%d" % b], w=["out%d" % i])
        s.barrier()
    return nc


_NC = None


def kernel(x, norm_mix_g, w_in, b_gate, gmlp_norm_g, w_spatial, b_spatial, w_proj_a, w_proj_b, w_out,
           norm_ffn_g, w_router, w_e_gate, w_e_up, w_e_down, norm_final_g):
    global _NC
    f = lambda a: np.ascontiguousarray(np.asarray(a, dtype=np.float32))
    shared = {
        "norm_mix_g": f(norm_mix_g).reshape(1, D),
        "w_in": f(w_in).reshape(D, INC),
        "b_gate": f(b_gate).reshape(16, 128),
        "gmlp_norm_g": f(gmlp_norm_g).reshape(1, 512),
        "w_spatial": f(w_spatial).reshape(8, 128, 128),
        "b_spatial": f(b_spatial).reshape(8, 128),
        "w_proj_a": f(w_proj_a).reshape(512, D),
        "w_proj_b": f(w_proj_b).reshape(512, D),
        "w_out": f(w_out).reshape(D, D),
        "norm_ffn_g": f(norm_ffn_g).reshape(1, D),
        "w_router": f(w_router).reshape(D, NE),
        "w_e_gate": f(w_e_gate).reshape(NE, D, D),
        "w_e_up": f(w_e_up).reshape(NE, D, D),
        "w_e_down": f(w_e_down).reshape(NE, D, D),
        "norm_final_g": f(norm_final_g).reshape(1, D),
    }
    x = f(x)
    if _NC is None:
        _NC = build()
    in_maps = []
    for c in range(8):
        m = dict(shared)
        m["x"] = x[c % 4]
        in_maps.append(m)
    res = run_bass_kernel_spmd(_NC, in_maps, core_ids=list(range(8)))
    if DEBUG:
        return res
    out = np.stack([np.asarray(res.results[c]["out"], dtype=np.float32).reshape(SEQ, D) for c in range(4)], axis=0)
    return out
```
